# Optimizing a Trainium2 kernel written in Bass

```python
import math
import jax
import jax.numpy as jnp
from jax import lax
import numpy as np


D_MODEL = 1024
BATCH = 8
SEQ = 2048
DEPTH = 4

GRID_W = 64
CTX_LEN = 256
Q_BLOCK = 128
ROPE_THETA = 10000.0
EPS = 1e-6
F32 = jnp.float32

DIFF_HEADS = 4
DIFF_HEAD_DIM = 48
DIFF_V_DIM = 96
GQA_Q_HEADS = 6
GQA_KV_HEADS = 2
GQA_HEAD_DIM = 64
MLSTM_HEADS = 4
MLSTM_HEAD_DIM = 64
MLSTM_CHUNK = 64
MLSTM_CONV = 3

DIFF_WIDTH = 384
GQA_WIDTH = 384
MLSTM_WIDTH = 256
MIX_WIDTH = 1024
IN_SPLITS = (384, 384, 384, 384, 128, 128, 512, 256, 256, 16)
IN_WIDTH = 2832

MOE_GROUPS = 4
MOE_EXPERTS_PER_GROUP = 8
MOE_EXPERTS = 32
MOE_TOP_K = 2
MOE_HIDDEN = 512
MOE_BLOCK = 128

kernel_name = 'hybrid_diffusion_block'


def rms_norm(x, g):
    xf = x.astype(F32)
    y = xf * lax.rsqrt(jnp.mean(xf * xf, axis=-1, keepdims=True) + EPS)
    return (y * g.astype(F32)).astype(x.dtype)


def modulate(h, shift, scale):
    return h * (1.0 + scale) + shift


def rope_1d(x, pos):
    nf = x.shape[-1] // 2
    freqs = ROPE_THETA ** (-jnp.arange(nf, dtype=F32) / nf)
    ang = pos.astype(F32)[:, None] * freqs
    ang = ang.reshape((ang.shape[0],) + (1,) * (x.ndim - 3) + (nf,))
    cos, sin = jnp.cos(ang), jnp.sin(ang)
    xf = x.astype(F32)
    x1, x2 = xf[..., :nf], xf[..., nf:]
    return jnp.concatenate([x1 * cos - x2 * sin, x2 * cos + x1 * sin], axis=-1).astype(x.dtype)


def axial_rope(x, rows, cols):
    half = x.shape[-1] // 2
    return jnp.concatenate([rope_1d(x[..., :half], rows), rope_1d(x[..., half:], cols)], axis=-1)


def to_query_blocks(q):
    nb = q.shape[-2] // Q_BLOCK
    qb = q.reshape(q.shape[:-2] + (nb, Q_BLOCK, q.shape[-1]))
    return jnp.moveaxis(qb, -3, 0)


def from_query_blocks(o):
    o = jnp.moveaxis(o, 0, -3)
    return o.reshape(o.shape[:-3] + (o.shape[-3] * o.shape[-2], o.shape[-1]))


def diff_attention(q1, q2, k1, k2, v, lam):
    scale = q1.shape[-1] ** -0.5

    def block(qs):
        a, b = qs
        p1 = jax.nn.softmax(jnp.einsum('bhqd,bhkd->bhqk', a, k1).astype(F32) * scale, axis=-1)
        p2 = jax.nn.softmax(jnp.einsum('bhqd,bhkd->bhqk', b, k2).astype(F32) * scale, axis=-1)
        return jnp.einsum('bhqk,bhkv->bhqv', (p1 - lam * p2).astype(v.dtype), v)

    return from_query_blocks(lax.map(block, (to_query_blocks(q1), to_query_blocks(q2))))


def gqa_attention(q, k, v):
    scale = q.shape[-1] ** -0.5

    def block(qb):
        p = jax.nn.softmax(jnp.einsum('bhgqd,bhkd->bhgqk', qb, k).astype(F32) * scale, axis=-1)
        return jnp.einsum('bhgqk,bhkd->bhgqd', p.astype(v.dtype), v)

    return from_query_blocks(lax.map(block, to_query_blocks(q)))


def centred_conv(x, w, b):
    pad = w.shape[0] // 2
    y = lax.conv_general_dilated(x, w[:, None, :], window_strides=(1,), padding=[(pad, pad)],
                                 dimension_numbers=('NWC', 'WIO', 'NWC'), feature_group_count=x.shape[-1])
    return y + b


def mlstm_zero_state(batch):
    return (jnp.zeros((batch, MLSTM_HEADS, MLSTM_HEAD_DIM, MLSTM_HEAD_DIM), F32),
            jnp.zeros((batch, MLSTM_HEADS, MLSTM_HEAD_DIM), F32),
            jnp.zeros((batch, MLSTM_HEADS), F32))


def mlstm_scan(q, k, v, log_i, log_f, state):
    B, H, T, _ = q.shape
    dv = v.shape[-1]
    L = MLSTM_CHUNK
    nc = T // L

    def chunks(a):
        return jnp.moveaxis(a.astype(F32).reshape((B, H, nc, L) + a.shape[3:]), 2, 0)

    tril = jnp.tril(jnp.ones((L, L), dtype=bool))

    def step(carry, inp):
        C, n, m = carry
        qc, kc, vc, li, lf = inp
        b = jnp.cumsum(lf, axis=-1)
        d_log = jnp.where(tril, b[..., :, None] - b[..., None, :] + li[..., None, :], -jnp.inf)
        inter = b + m[..., None]
        m_t = jnp.maximum(inter, jnp.max(d_log, axis=-1))
        w = jnp.exp(d_log - m_t[..., None]) * jnp.einsum('bhtd,bhsd->bhts', qc, kc)
        a = jnp.exp(inter - m_t)
        num = jnp.einsum('bhts,bhsv->bhtv', w, vc) + a[..., None] * jnp.einsum('bhvd,bhtd->bhtv', C, qc)
        den = jnp.sum(w, axis=-1) + a * jnp.einsum('bhd,bhtd->bht', n, qc)
        h = num / jnp.maximum(jnp.abs(den), jnp.exp(-m_t))[..., None]
        b_end = b[..., -1]
        g = b_end[..., None] - b + li
        m_new = jnp.maximum(b_end + m, jnp.max(g, axis=-1))
        ws = jnp.exp(g - m_new[..., None])
        a_end = jnp.exp(b_end + m - m_new)
        C_new = a_end[..., None, None] * C + jnp.einsum('bhs,bhsv,bhsd->bhvd', ws, vc, kc)
        n_new = a_end[..., None] * n + jnp.einsum('bhs,bhsd->bhd', ws, kc)
        return (C_new, n_new, m_new), h

    state, hs = lax.scan(step, state, (chunks(q), chunks(k), chunks(v), chunks(log_i), chunks(log_f)))
    h = jnp.moveaxis(hs, 0, 2).reshape(B, H, T, dv)
    return h.astype(v.dtype), state


def hierarchical_moe(xf, wg, bg, we, be, w1, w3, w2):
    N, D = xf.shape
    E = w1.shape[0]
    g_prob = jax.nn.softmax((xf @ wg).astype(F32) + bg.astype(F32), axis=-1)
    g_top, g_idx = lax.top_k(g_prob, 1)
    e_logits = ((xf @ we).astype(F32) + be.astype(F32)).reshape(N, MOE_GROUPS, MOE_EXPERTS_PER_GROUP)
    e_in_group = jnp.take_along_axis(e_logits, g_idx[:, :, None], axis=1)[:, 0]
    e_top, e_sub = lax.top_k(e_in_group, MOE_TOP_K)
    gates = jax.nn.softmax(e_top, axis=-1) * g_top
    expert = (g_idx * MOE_EXPERTS_PER_GROUP + e_sub).astype(jnp.int32)
    NK = N * MOE_TOP_K
    e_flat = expert.reshape(-1)
    g_flat = gates.reshape(-1)
    tok_flat = jnp.repeat(jnp.arange(N, dtype=jnp.int32), MOE_TOP_K)
    order = jnp.argsort(e_flat)
    e_sorted, tok_sorted, g_sorted = e_flat[order], tok_flat[order], g_flat[order]
    counts = jnp.zeros((E,), jnp.int32).at[e_flat].add(1)
    padded = (counts + MOE_BLOCK - 1) // MOE_BLOCK * MOE_BLOCK
    pad_end = jnp.cumsum(padded)
    pad_start = pad_end - padded
    start = jnp.cumsum(counts) - counts
    dest = pad_start[e_sorted] + jnp.arange(NK, dtype=jnp.int32) - start[e_sorted]
    P = -(-(NK + E * MOE_BLOCK) // MOE_BLOCK) * MOE_BLOCK
    row_tok = jnp.full((P,), N, jnp.int32).at[dest].set(tok_sorted)
    row_gate = jnp.zeros((P,), F32).at[dest].set(g_sorted)
    n_blk = P // MOE_BLOCK
    blk_start = jnp.arange(n_blk, dtype=jnp.int32) * MOE_BLOCK
    blk_expert = jnp.minimum(jnp.sum(pad_end[None, :] <= blk_start[:, None], axis=-1), E - 1).astype(jnp.int32)
    x_rows = jnp.concatenate([xf, jnp.zeros((1, D), xf.dtype)], axis=0)[row_tok].reshape(n_blk, MOE_BLOCK, D)

    def expert_block(args):
        xb, e = args
        return (jax.nn.silu(xb @ w1[e]) * (xb @ w3[e])) @ w2[e]

    y = lax.map(expert_block, (x_rows, blk_expert)).reshape(P, D)
    out = jnp.zeros((N + 1, D), y.dtype).at[row_tok].add(y * row_gate[:, None].astype(y.dtype))
    return out[:N]


def hybrid_layer(x, ctx, c, c_ctx, rows, cols, lam_init, update_ctx,
                 norm1_g, norm2_g, w_mod, b_mod, w_in, w_out,
                 diff_q_norm, diff_k_norm, diff_lambda, diff_subln, gqa_q_norm, gqa_k_norm,
                 mlstm_conv_w, mlstm_conv_b, mlstm_gate_b, mlstm_head_norm,
                 moe_wg, moe_bg, moe_we, moe_be, moe_w1, moe_w3, moe_w2):
    B, T, D = x.shape
    C = ctx.shape[1]
    sh1, sc1, g1, sh2, sc2, g2 = jnp.split((jax.nn.silu(c) @ w_mod + b_mod)[:, None, :], 6, axis=-1)
    sh1c, sc1c, g1c, sh2c, sc2c, g2c = jnp.split(jax.nn.silu(c_ctx) @ w_mod + b_mod, 6, axis=-1)
    split_at = [int(i) for i in np.cumsum(IN_SPLITS)[:-1]]
    dq_l, dk_l, dv_l, gq_l, gk_l, gv_l, mqk_l, mv_l, mo_l, mg_l = jnp.split(
        modulate(rms_norm(x, norm1_g), sh1, sc1) @ w_in, split_at, axis=-1)
    dq_c, dk_c, dv_c, gq_c, gk_c, gv_c, mqk_c, mv_c, mo_c, mg_c = jnp.split(
        modulate(rms_norm(ctx, norm1_g), sh1c, sc1c) @ w_in, split_at, axis=-1)

    def heads(p, n, d):
        return p.reshape(p.shape[0], p.shape[1], n, d).transpose(0, 2, 1, 3)

    def qk_heads(p, n, d, g, rope):
        y = rms_norm(p.reshape(p.shape[0], p.shape[1], n, d), g)
        if rope:
            y = axial_rope(y, rows, cols)
        return y.transpose(0, 2, 1, 3)

    def diff_qk(p, g, rope):
        y = rms_norm(p.reshape(p.shape[0], p.shape[1], DIFF_HEADS, 2, DIFF_HEAD_DIM), g)
        if rope:
            y = axial_rope(y, rows, cols)
        y = y.transpose(3, 0, 2, 1, 4)
        return y[0], y[1]

    def cat(a_c, a_l):
        return jnp.concatenate([a_c, a_l], axis=2)

    lam = (jnp.exp(jnp.sum(diff_lambda[0] * diff_lambda[1]).astype(F32))
           - jnp.exp(jnp.sum(diff_lambda[2] * diff_lambda[3]).astype(F32)) + lam_init)
    k1_c, k2_c = diff_qk(dk_c, diff_k_norm, False)
    k1_l, k2_l = diff_qk(dk_l, diff_k_norm, True)
    q1_l, q2_l = diff_qk(dq_l, diff_q_norm, True)
    dv_ch = heads(dv_c, DIFF_HEADS, DIFF_V_DIM)
    dv_lh = heads(dv_l, DIFF_HEADS, DIFF_V_DIM)

    def diff_out(o):
        o = rms_norm(o.transpose(0, 2, 1, 3), diff_subln) * (1.0 - lam_init)
        return o.reshape(o.shape[0], o.shape[1], DIFF_WIDTH)

    diff_lat = diff_out(diff_attention(q1_l, q2_l, cat(k1_c, k1_l), cat(k2_c, k2_l), cat(dv_ch, dv_lh), lam))

    def group_q(q):
        return q.reshape(q.shape[0], GQA_KV_HEADS, GQA_Q_HEADS // GQA_KV_HEADS, q.shape[2], GQA_HEAD_DIM)

    def gqa_out(o):
        o = o.reshape(o.shape[0], GQA_Q_HEADS, o.shape[3], GQA_HEAD_DIM).transpose(0, 2, 1, 3)
        return o.reshape(o.shape[0], o.shape[1], GQA_WIDTH)

    gk_ch = qk_heads(gk_c, GQA_KV_HEADS, GQA_HEAD_DIM, gqa_k_norm, False)
    gk_lh = qk_heads(gk_l, GQA_KV_HEADS, GQA_HEAD_DIM, gqa_k_norm, True)
    gv_ch = heads(gv_c, GQA_KV_HEADS, GQA_HEAD_DIM)
    gv_lh = heads(gv_l, GQA_KV_HEADS, GQA_HEAD_DIM)
    gq_lh = group_q(qk_heads(gq_l, GQA_Q_HEADS, GQA_HEAD_DIM, gqa_q_norm, True))
    gqa_lat = gqa_out(gqa_attention(gq_lh, cat(gk_ch, gk_lh), cat(gv_ch, gv_lh)))

    def mlstm_inputs(qk, v, gates):
        qk = jax.nn.silu(centred_conv(qk, mlstm_conv_w, mlstm_conv_b))
        q, k = jnp.split(qk, 2, axis=-1)
        q = heads(q, MLSTM_HEADS, MLSTM_HEAD_DIM)
        k = heads(k, MLSTM_HEADS, MLSTM_HEAD_DIM) * (MLSTM_HEAD_DIM ** -0.5)
        v = heads(v, MLSTM_HEADS, MLSTM_HEAD_DIM)
        gt = (gates + mlstm_gate_b).astype(F32).reshape(gates.shape[0], gates.shape[1], 4, MLSTM_HEADS)
        gt = gt.transpose(2, 0, 3, 1)
        return q, k, v, gt[0], jax.nn.log_sigmoid(gt[1]), gt[2], jax.nn.log_sigmoid(gt[3])

    def flip(a):
        return jnp.flip(a, axis=2)

    def mlstm_out(h, o):
        h = rms_norm(h.transpose(0, 2, 1, 3), mlstm_head_norm.reshape(MLSTM_HEADS, MLSTM_HEAD_DIM))
        h = h * jax.nn.sigmoid(o).reshape(h.shape)
        return h.reshape(h.shape[0], h.shape[1], MLSTM_WIDTH)

    qc, kc, vc, ifc, ffc, ibc, fbc = mlstm_inputs(mqk_c, mv_c, mg_c)
    ql, kl, vl, ifl, ffl, ibl, fbl = mlstm_inputs(mqk_l, mv_l, mg_l)
    zero = mlstm_zero_state(B)
    hf_c, st_f = mlstm_scan(qc, kc, vc, ifc, ffc, zero)
    hb_c, st_b = mlstm_scan(flip(qc), flip(kc), flip(vc), flip(ibc), flip(fbc), zero)
    hf_l, _ = mlstm_scan(ql, kl, vl, ifl, ffl, st_f)
    hb_l, _ = mlstm_scan(flip(ql), flip(kl), flip(vl), flip(ibl), flip(fbl), st_b)
    mlstm_lat = mlstm_out(hf_l + flip(hb_l), mo_l)

    x = x + g1 * (jnp.concatenate([diff_lat, gqa_lat, mlstm_lat], axis=-1) @ w_out)
    f_l = modulate(rms_norm(x, norm2_g), sh2, sc2)
    moe_args = (moe_wg, moe_bg, moe_we, moe_be, moe_w1, moe_w3, moe_w2)
    if update_ctx:
        q1_c, q2_c = diff_qk(dq_c, diff_q_norm, False)
        diff_ctx = diff_out(diff_attention(q1_c, q2_c, k1_c, k2_c, dv_ch, lam))
        gq_ch = group_q(qk_heads(gq_c, GQA_Q_HEADS, GQA_HEAD_DIM, gqa_q_norm, False))
        gqa_ctx = gqa_out(gqa_attention(gq_ch, gk_ch, gv_ch))
        mlstm_ctx = mlstm_out(hf_c + flip(hb_c), mo_c)
        ctx = ctx + g1c * (jnp.concatenate([diff_ctx, gqa_ctx, mlstm_ctx], axis=-1) @ w_out)
        f_c = modulate(rms_norm(ctx, norm2_g), sh2c, sc2c)
        y = hierarchical_moe(jnp.concatenate([f_l.reshape(-1, D), f_c.reshape(-1, D)], axis=0), *moe_args)
        x = x + g2 * y[:B * T].reshape(B, T, D)
        ctx = ctx + g2c * y[B * T:].reshape(B, C, D)
    else:
        x = x + g2 * hierarchical_moe(f_l.reshape(-1, D), *moe_args).reshape(B, T, D)
    return x, ctx


def setup_inputs(seed: int = 0) -> dict:
    key = jax.random.key(seed)
    ks = jax.random.split(key, 27)
    D = D_MODEL
    L = DEPTH

    def nrm(k, shape, scale):
        return scale * jax.random.normal(k, shape, F32)

    fb = jnp.linspace(3.0, 6.0, MLSTM_HEADS, dtype=F32)
    zb = jnp.zeros((MLSTM_HEADS,), F32)
    gate_base = jnp.concatenate([zb, fb, zb, fb])
    return {
        'x': nrm(ks[0], (BATCH, SEQ, D), 1.0),
        'c': nrm(ks[1], (BATCH, D), 1.0),
        'ctx': nrm(ks[2], (BATCH, CTX_LEN, D), 1.0),
        'c_ctx': nrm(ks[3], (D,), 1.0),
        'norm1_g': 1.0 + nrm(ks[4], (L, D), 0.02),
        'norm2_g': 1.0 + nrm(ks[5], (L, D), 0.02),
        'w_mod': nrm(ks[6], (L, D, 6 * D), 0.5 * D ** -0.5),
        'b_mod': nrm(ks[7], (L, 6 * D), 0.02),
        'w_in': nrm(ks[8], (L, D, IN_WIDTH), D ** -0.5),
        'w_out': nrm(ks[9], (L, MIX_WIDTH, D), MIX_WIDTH ** -0.5),
        'diff_q_norm': 1.0 + nrm(ks[10], (L, DIFF_HEAD_DIM), 0.02),
        'diff_k_norm': 1.0 + nrm(ks[11], (L, DIFF_HEAD_DIM), 0.02),
        'diff_lambda': nrm(ks[12], (L, 4, DIFF_HEAD_DIM), 0.1),
        'diff_subln': 1.0 + nrm(ks[13], (L, DIFF_V_DIM), 0.02),
        'gqa_q_norm': 1.0 + nrm(ks[14], (L, GQA_HEAD_DIM), 0.02),
        'gqa_k_norm': 1.0 + nrm(ks[15], (L, GQA_HEAD_DIM), 0.02),
        'mlstm_conv_w': nrm(ks[16], (L, MLSTM_CONV, 2 * MLSTM_WIDTH), MLSTM_CONV ** -0.5),
        'mlstm_conv_b': nrm(ks[17], (L, 2 * MLSTM_WIDTH), 0.02),
        'mlstm_gate_b': gate_base + nrm(ks[18], (L, 4 * MLSTM_HEADS), 0.1),
        'mlstm_head_norm': 1.0 + nrm(ks[19], (L, MLSTM_WIDTH), 0.02),
        'moe_wg': nrm(ks[20], (L, D, MOE_GROUPS), D ** -0.5),
        'moe_bg': nrm(ks[21], (L, MOE_GROUPS), 0.01),
        'moe_we': nrm(ks[22], (L, D, MOE_EXPERTS), D ** -0.5),
        'moe_be': nrm(ks[23], (L, MOE_EXPERTS), 0.01),
        'moe_w1': nrm(ks[24], (L, MOE_EXPERTS, D, MOE_HIDDEN), D ** -0.5),
        'moe_w3': nrm(ks[25], (L, MOE_EXPERTS, D, MOE_HIDDEN), D ** -0.5),
        'moe_w2': nrm(ks[26], (L, MOE_EXPERTS, MOE_HIDDEN, D), MOE_HIDDEN ** -0.5),
    }


def reference(x, c, ctx, c_ctx, norm1_g, norm2_g, w_mod, b_mod, w_in, w_out,
              diff_q_norm, diff_k_norm, diff_lambda, diff_subln, gqa_q_norm, gqa_k_norm,
              mlstm_conv_w, mlstm_conv_b, mlstm_gate_b, mlstm_head_norm,
              moe_wg, moe_bg, moe_we, moe_be, moe_w1, moe_w3, moe_w2):
    n_rows = x.shape[1] // GRID_W
    rows = jnp.repeat(jnp.arange(n_rows, dtype=jnp.int32), GRID_W)
    cols = jnp.tile(jnp.arange(GRID_W, dtype=jnp.int32), n_rows)
    for l in range(DEPTH):
        lam_init = 0.8 - 0.6 * math.exp(-0.3 * l)
        x, ctx = hybrid_layer(x, ctx, c, c_ctx, rows, cols, lam_init, l < DEPTH - 1,
                              norm1_g[l], norm2_g[l], w_mod[l], b_mod[l], w_in[l], w_out[l],
                              diff_q_norm[l], diff_k_norm[l], diff_lambda[l], diff_subln[l],
                              gqa_q_norm[l], gqa_k_norm[l],
                              mlstm_conv_w[l], mlstm_conv_b[l], mlstm_gate_b[l], mlstm_head_norm[l],
                              moe_wg[l], moe_bg[l], moe_we[l], moe_be[l], moe_w1[l], moe_w3[l], moe_w2[l])
    return x
```

```python
import math
import numpy as np
import concourse.bass as bass
import concourse.mybir as mybir
from concourse.bass_utils import run_bass_kernel_spmd

F32 = mybir.dt.float32
BF16 = mybir.dt.bfloat16
I32 = mybir.dt.int32
ALU = mybir.AluOpType
AF = mybir.ActivationFunctionType
AX = mybir.AxisListType

D = 1024
T = 2048
C = 256
NT = 18
DEPTH = 4
EPS = 1e-6
NE = 32
CHUNKS = [[0, 1, 2, 3], [4, 5, 6, 7], [8, 9, 10, 11], [12, 13, 14, 15], [16, 17]]
N_LAYERS_PER_LAUNCH = 4
PHASES = {'mod', 'A1', 'A2', 'B'}
SKIPV = 1.0e6
DBG = {}


class Prog:
    def __init__(self, nc, ndma=56):
        self.nc = nc
        self.E = {'pe': nc.tensor, 'act': nc.scalar, 'dve': nc.vector, 'pool': nc.gpsimd, 'sp': nc.sync}
        self.sem = {k: nc.alloc_semaphore(name='s_' + k) for k in self.E}
        self.cnt = {k: 0 for k in self.E}
        self.dsem = [nc.alloc_semaphore(name='d%d' % i) for i in range(ndma)]
        self.dcnt = [0] * ndma
        self.dnext = 0
        self.dnext_pool = 0
        self.lastw = {}
        self.rd = {}
        self.waited = {k: {} for k in self.E}
        self.nwait = 0
        self.mute = False

    def _wait(self, eng, dep):
        s, v = dep
        key = id(s)
        if eng == 'pe' and s is self.sem['pe']:
            return
        if self.waited[eng].get(key, 0) >= v:
            return
        self.E[eng].wait_ge(s, v)
        self.nwait += 1
        self.waited[eng][key] = v

    def _deps(self, eng, reads, writes):
        for k in reads:
            if k in self.lastw:
                self._wait(eng, self.lastw[k])
        for k in writes:
            if k in self.lastw:
                self._wait(eng, self.lastw[k])
            for d in self.rd.get(k, ()):
                self._wait(eng, d)

    def _mark(self, tok, reads, writes):
        for k in writes:
            self.lastw[k] = tok
            self.rd[k] = []
        for k in reads:
            if k not in writes:
                self.rd.setdefault(k, []).append(tok)

    @staticmethod
    def _excl(reads, writes):
        pr = [k for k in reads if k.startswith('pb')]
        if pr:
            reads = [k for k in reads if not k.startswith('pb')]
            writes = list(writes) + [k for k in pr if k not in writes]
        return reads, writes

    def op(self, eng, fn, reads=(), writes=()):
        if self.mute:
            return
        reads, writes = self._excl(reads, writes)
        self._deps(eng, reads, writes)
        inst = fn(self.E[eng])
        self.cnt[eng] += 1
        inst.then_inc(self.sem[eng], 1)
        self._mark((self.sem[eng], self.cnt[eng]), reads, writes)

    def dma(self, eng, out, in_, reads=(), writes=(), **kw):
        if self.mute:
            return
        half = 12
        if eng == 'pool':
            i = half + self.dnext_pool
            self.dnext_pool = (self.dnext_pool + 1) % (len(self.dsem) - half)
        else:
            i = self.dnext
            self.dnext = (self.dnext + 1) % half
        if self.dcnt[i] > 0:
            self._wait(eng, (self.dsem[i], self.dcnt[i]))
        self._deps(eng, reads, writes)
        inst = self.E[eng].dma_start(out=out, in_=in_, **kw)
        self.dcnt[i] += 16
        inst.then_inc(self.dsem[i], 16)
        self._mark((self.dsem[i], self.dcnt[i]), reads, writes)

    def idma(self, out, out_off, in_, in_off, reads=(), writes=(), bounds=None):
        if self.mute:
            return
        eng = 'pool'
        half = 12
        i = half + self.dnext_pool
        self.dnext_pool = (self.dnext_pool + 1) % (len(self.dsem) - half)
        if self.dcnt[i] > 0:
            self._wait(eng, (self.dsem[i], self.dcnt[i]))
        self._deps(eng, reads, writes)
        if bounds is None:
            inst = self.E[eng].indirect_dma_start(out=out, out_offset=out_off, in_=in_, in_offset=in_off)
        else:
            inst = self.E[eng].indirect_dma_start(out=out, out_offset=out_off, in_=in_, in_offset=in_off, bounds_check=bounds, oob_is_err=False)
        self.dcnt[i] += 16
        inst.then_inc(self.dsem[i], 16)
        self._mark((self.dsem[i], self.dcnt[i]), reads, writes)

    def barrier(self):
        for e in self.E:
            for f in self.E:
                if f != e and self.cnt[f] > 0:
                    self._wait(e, (self.sem[f], self.cnt[f]))
            for i, s in enumerate(self.dsem):
                if self.dcnt[i] > 0:
                    self._wait(e, (s, self.dcnt[i]))
        self.lastw = {}
        self.rd = {}

    def finish(self, eng='sp'):
        for f in self.E:
            if f != eng and self.cnt[f] > 0:
                self._wait(eng, (self.sem[f], self.cnt[f]))
        for i, s in enumerate(self.dsem):
            if self.dcnt[i] > 0:
                self._wait(eng, (s, self.dcnt[i]))


class Arena:
    def __init__(self, nc):
        self.nc = nc
        self.cur = (nc.sbuf_base + 63) // 64 * 64
        self.top = nc.sbuf_top
        self.n = 0

    def alloc(self, shape, dtype):
        per = 1
        for s in shape[1:]:
            per *= s
        per *= 2 if dtype == BF16 else 4
        per = (per + 63) // 64 * 64
        off = self.cur
        self.cur += per
        assert self.cur <= self.top, "SBUF overflow: need %d more bytes" % (self.cur - self.top)
        self.n += 1
        return self.nc.alloc_sbuf_tensor_at("sb%d" % self.n, list(shape), dtype, offset=off).ap()

    def mark(self):
        return self.cur

    def reset(self, m):
        self.cur = m


def bc(ap, shape):
    return ap.to_broadcast(list(shape))


def build_program(NL, do_ctx_last):
    DBG.pop('bnd', None)
    nc = bass.Bass("TRN2", target_bir_lowering=False)

    def din(name, shape):
        return nc.dram_tensor(name, list(shape), F32, kind="ExternalInput").ap()

    x_d = din("x", [T, D])
    ctx_d = din("ctx", [C, D])
    cc_d = din("cc", [2, D])
    n1_d = din("norm1_g", [NL, D])
    n2_d = din("norm2_g", [NL, D])
    wmod_d = din("w_mod", [NL, D, 6 * D])
    bmod_d = din("b_mod", [NL, 6 * D])
    win_d = din("w_in", [NL, D, 2832])
    wout_d = din("w_out", [NL, D, D])
    dqn_d = din("diff_q_norm", [NL, 48])
    dkn_d = din("diff_k_norm", [NL, 48])
    dlam_d = din("diff_lambda", [NL, 4, 48])
    dsub_d = din("diff_subln", [NL, 96])
    gqn_d = din("gqa_q_norm", [NL, 64])
    gkn_d = din("gqa_k_norm", [NL, 64])
    cw_d = din("mlstm_conv_w", [NL, 3, 512])
    cb_d = din("mlstm_conv_b", [NL, 512])
    gtb_d = din("mlstm_gate_b", [NL, 16])
    hng_d = din("mlstm_head_norm", [NL, 256])
    wg_d = din("moe_wg", [NL, D, 4])
    bg_d = din("moe_bg", [NL, 4])
    we_d = din("moe_we", [NL, D, 32])
    be_d = din("moe_be", [NL, 32])
    w1_d = din("moe_w1", [NL, NE, D, 512])
    w3_d = din("moe_w3", [NL, NE, D, 512])
    w2_d = din("moe_w2", [NL, NE, 512, D])
    lamc_d = din("lamc", [NL, 2])
    identf_d = din("identf", [128, 128])
    trif_d = din("trif", [128, 128])
    trib_d = din("trib", [128, 128])
    cos64_d = din("cos64", [128, NT, 64])
    sin64_d = din("sin64", [128, NT, 64])
    cos48_d = din("cos48", [128, NT, 48])
    sin48_d = din("sin48", [128, NT, 48])
    xo_d = nc.dram_tensor("xo", [T, D], F32, kind="ExternalOutput").ap()
    co_d = nc.dram_tensor("co", [C, D], F32, kind="ExternalOutput").ap()
    hmd_d = nc.dram_tensor("hmd", [NT * 128, 256], BF16, kind="Internal").ap()
    NBMAX = NT * 2 + NE
    xg_d = nc.dram_tensor("xg", [NBMAX * 128, D], BF16, kind="Internal").ap()
    yg_d = nc.dram_tensor("yg", [NBMAX * 128, D], F32, kind="Internal").ap()
    tris_d = din("tris", [128, 128])
    thr_d = din("thr", [128, 104])
    base1_d = din("base1", [128, 8])
    base2_d = din("base2", [128, 4])

    P = Prog(nc)
    A = Arena(nc)
    PB = [nc.alloc_psum_tensor("pb%d" % i, [128, 512], F32).ap() for i in range(8)]
    PBH = [p.bitcast(BF16) for p in PB]

    def op(eng, fn, r=(), w=()):
        P.op(eng, fn, r, w)

    def mm(out, lhsT, rhs, start, stop, r, w, skip=False):
        P.op('pe', lambda e: e.matmul(out, lhsT=lhsT, rhs=rhs, start=start, stop=stop, skip_group_check=skip), r, w)

    xs = A.alloc([128, NT, D], F32)
    identb = A.alloc([128, 128], BF16)
    identf = A.alloc([128, 128], F32)
    trif = A.alloc([128, 128], F32)
    trib = A.alloc([128, 128], F32)
    ones64 = A.alloc([128, 128], F32)
    trisb = A.alloc([128, 128], BF16)
    onesb = A.alloc([128, 128], BF16)
    thr = A.alloc([128, 104], F32)
    base1 = A.alloc([128, 8], F32)
    base2 = A.alloc([128, 4], F32)
    cos64 = A.alloc([128, NT, 64], BF16)
    sin64 = A.alloc([128, NT, 64], BF16)
    cos48 = A.alloc([128, NT, 48], BF16)
    sin48 = A.alloc([128, NT, 48], BF16)
    scol = A.alloc([128, 2, 8], BF16)
    gb = A.alloc([128, 2, 2, D], BF16)
    modA = A.alloc([128, 2, 8, 2], F32)
    modB = A.alloc([128, 2, 8, 2], F32)
    sv = A.alloc([128, 900], F32)
    cwc = A.alloc([128, 3, 4], F32)
    cbc = A.alloc([128, 4], F32)
    kc = A.alloc([128, 8], F32)
    tiny = A.alloc([128, 128], F32)
    SV_DQN, SV_DKN, SV_DSUB, SV_GQN, SV_GKN, SV_HNG, SV_GTB, SV_RB, SV_LAMC, SV_DLAM = 0, 48, 96, 192, 256, 320, 576, 592, 628, 640
    pers_mark = A.mark()
    DBG.update(xs=xs, modA=modA, modB=modB, gb=gb, sv=sv, scol=scol, hmd=hmd_d)

    ld_mark = A.mark()
    stg = A.alloc([128, NT, 64], F32)
    for (dst, src, d) in ((cos64, cos64_d, 64), (sin64, sin64_d, 64), (cos48, cos48_d, 48), (sin48, sin48_d, 48)):
        P.dma('pool', dst, src, writes=['tbl'])
    P.dma('sp', identf, identf_d, writes=['identf'])
    P.dma('pool', identb, identf_d, writes=['identb'])
    P.dma('sp', trif, trif_d, writes=['trif'])
    P.dma('sp', trib, trib_d, writes=['trib'])
    P.dma('pool', trisb, tris_d, writes=['trisb'])
    P.dma('sp', thr, thr_d, writes=['thr'])
    P.dma('sp', base1, base1_d, writes=['base'])
    P.dma('sp', base2, base2_d, writes=['base'])
    op('pool', lambda e: e.memset(onesb, 1.0), w=['onesb'])
    op('pool', lambda e: e.memset(ones64, 1.0), w=['ones64'])
    op('pool', lambda e: e.memset(kc[:, 0:1], EPS), w=['kc'])
    op('pool', lambda e: e.memset(kc[:, 1:2], math.log(0.125)), w=['kc'])
    op('pool', lambda e: e.memset(kc[:, 2:3], 1.0), w=['kc'])
    op('pool', lambda e: e.memset(kc[:, 3:4], 0.0), w=['kc'])
    xr = x_d.rearrange("(t p) d -> p t d", p=128)
    for i in range(4):
        P.dma('sp', xs[:, 4 * i:4 * i + 4, :], xr[:, 4 * i:4 * i + 4, :], writes=['xs%d' % t for t in range(4 * i, 4 * i + 4)])
    P.dma('sp', xs[:, 16:18, :], ctx_d.rearrange("(t p) d -> p t d", p=128), writes=['xs16', 'xs17'])
    ccf = A.alloc([128, 2, 8], F32)
    for r_i in range(2):
        P.dma('sp', ccf[:, r_i, :], cc_d[r_i].rearrange("(k p) -> p k", p=128), writes=['ccf'], allow_slow_non_contiguous=True)
    op('act', lambda e: e.activation(out=scol, in_=ccf, func=AF.Silu), r=['ccf'], w=['scol'])
    zt = A.alloc([128, 4, D], BF16)
    op('pool', lambda e: e.memset(zt, 0.0), w=['zt'])
    xgz = xg_d.rearrange("(g b p) d -> g p b d", p=128, b=4)
    for gi_ in range(NBMAX // 4):
        P.dma('sp', xgz[gi_], zt, reads=['zt'], writes=['xgz%d' % gi_])
    A.reset(ld_mark)
    P.barrier()

    eps_ap = kc[:, 0:1]
    ln8_ap = kc[:, 1:2]

    def rstd_from_ss(ss_ap, n, dim, rk, wk):
        op('act', lambda e: e.activation(out=ss_ap, in_=ss_ap, func=AF.Ln, scale=1.0 / dim, bias=eps_ap), r=rk, w=wk)
        op('act', lambda e: e.activation(out=ss_ap, in_=ss_ap, func=AF.Exp, scale=-0.5), r=wk, w=wk)

    def norm_T(dst, tiles, which, scr):
        junk, xn, ss = scr[:3]
        jk = scr[3] if len(scr) > 3 else 'nT_junk'
        for i, t in enumerate(tiles):
            r = 0 if t < 16 else 1
            op('pool', lambda e: e.memset(ss, 0.0), w=['nT_ss'])
            op('act', lambda e: e.activation(out=junk, in_=xs[:, t, :], func=AF.Square, accum_out=ss), r=['xs%d' % t, 'nT_ss'], w=[jk, 'nT_ss'])
            rstd_from_ss(ss, 1, D, ['nT_ss'], ['nT_ss'])
            op('dve', lambda e: e.tensor_scalar(out=xn, in0=xs[:, t, :], scalar1=ss, scalar2=None, op0=ALU.mult), r=['xs%d' % t, 'nT_ss'], w=['nT_xn'])
            pst = PBH[7].rearrange("p (k n) -> p k n", k=8)
            for k in range(8):
                op('pe', lambda e: e.transpose(pst[:, k, :], xn[:, k * 128:(k + 1) * 128], identb), r=['nT_xn', 'identb'], w=['pb7'])
            for k in range(8):
                eng = 'dve' if t % 2 == 0 else 'act'
                o = dst[:, k, i * 128:(i + 1) * 128]
                if eng == 'dve':
                    op('dve', lambda e: e.tensor_scalar(out=o, in0=pst[:, k, :], scalar1=modA[:, which, k, r:r + 1], scalar2=modB[:, which, k, r:r + 1], op0=ALU.mult, op1=ALU.add), r=['pb7', 'mod'], w=['hT'])
                else:
                    op('act', lambda e: e.activation(out=o, in_=pst[:, k, :], func=AF.Identity, scale=modA[:, which, k, r:r + 1], bias=modB[:, which, k, r:r + 1]), r=['pb7', 'mod'], w=['hT'])

    def qknr(ps_ap, U, d, gain, cos_t, sin_t, out_ap, scr, psk, split=None):
        s1, s2, s3, ss = scr
        q4 = d // 4
        s1v = s1[:, 0:U * d].rearrange("p (u d) -> p u d", u=U)
        s2v = s2[:, 0:U * d].rearrange("p (u d) -> p u d", u=U)
        s3v = s3[:, 0:U * d].rearrange("p (u d) -> p u d", u=U)
        ssv = ss[:, 0:U]
        psv = ps_ap.rearrange("p (u d) -> p u d", u=U)
        op('act', lambda e: e.activation(out=s1[:, 0:U * d], in_=ps_ap, func=AF.Square), r=[psk], w=['q_s1'])
        op('dve', lambda e: e.tensor_reduce(out=ssv, in_=s1v, axis=AX.X, op=ALU.add), r=['q_s1'], w=['q_ss'])
        rstd_from_ss(ssv, U, d, ['q_ss'], ['q_ss'])
        op('dve', lambda e: e.tensor_tensor(out=s1v, in0=psv, in1=bc(ssv.unsqueeze(2), [128, U, d]), op=ALU.mult), r=[psk, 'q_ss', 'q_s1'], w=['q_s1'])
        op('pool', lambda e: e.tensor_tensor(out=s1v, in0=s1v, in1=bc(gain.unsqueeze(1), [128, U, d]), op=ALU.mult), r=['q_s1', 'sv'], w=['q_s1'])
        op('pool', lambda e: e.tensor_tensor(out=s2v, in0=s1v, in1=bc(cos_t.unsqueeze(1), [128, U, d]), op=ALU.mult), r=['q_s1', 'tbl'], w=['q_s2'])
        x5 = s1[:, 0:U * d].rearrange("p (u a b c) -> p u a b c", u=U, a=2, b=2)
        t5 = s3[:, 0:U * d].rearrange("p (u a b c) -> p u a b c", u=U, a=2, b=2)
        sn5 = sin_t.rearrange("p (a b c) -> p a b c", a=2, b=2)
        for b_ in range(2):
            op('pool', lambda e: e.tensor_tensor(out=t5[:, :, :, b_, :], in0=x5[:, :, :, 1 - b_, :], in1=bc(sn5[:, :, b_, :].unsqueeze(1), [128, U, 2, q4]), op=ALU.mult), r=['q_s1', 'tbl'], w=['q_s3'])
        if split is not None:
            s2o = s2v.rearrange("p (k j) d -> p k j d", k=split)
            s3o = s3v.rearrange("p (k j) d -> p k j d", k=split)
        else:
            s2o, s3o = s2v, s3v
        op('dve', lambda e: e.tensor_tensor(out=out_ap, in0=s2o, in1=s3o, op=ALU.add), r=['q_s2', 'q_s3'], w=['q_out'])

    for l in range(NL):
        last = (l == NL - 1)
        upd_ctx = (not last) or do_ctx_last
        lay_mark = A.mark()
        for (off, src, n) in ((SV_DQN, dqn_d[l], 48), (SV_DKN, dkn_d[l], 48), (SV_DSUB, dsub_d[l], 96), (SV_GQN, gqn_d[l], 64),
                              (SV_GKN, gkn_d[l], 64), (SV_HNG, hng_d[l], 256), (SV_GTB, gtb_d[l], 16), (SV_RB, bg_d[l], 4),
                              (SV_RB + 4, be_d[l], 32), (SV_LAMC, lamc_d[l], 2)):
            P.dma('sp', sv[:, off:off + n], src.partition_broadcast(128), writes=['sv'])
        P.dma('sp', sv[:, SV_DLAM:SV_DLAM + 192], dlam_d[l].rearrange("a b -> (a b)").partition_broadcast(128), writes=['sv'])
        for j_ in range(3):
            P.dma('sp', cwc[:, j_, :], cw_d[l, j_].rearrange("(ct p) -> p ct", p=128), writes=['cwc'], allow_slow_non_contiguous=True)
        P.dma('sp', cbc, cb_d[l].rearrange("(ct p) -> p ct", p=128), writes=['cwc'], allow_slow_non_contiguous=True)
        dl = sv[:, SV_DLAM:SV_DLAM + 192].rearrange("p (a b) -> p a b", a=4)
        op('dve', lambda e: e.tensor_tensor(out=tiny[:, 0:96].rearrange("p (a b) -> p a b", a=2), in0=dl[:, 0:4:2, :], in1=dl[:, 1:4:2, :], op=ALU.mult), r=['sv'], w=['tiny'])
        op('dve', lambda e: e.tensor_reduce(out=tiny[:, 96:98], in_=tiny[:, 0:96].rearrange("p (a b) -> p a b", a=2), axis=AX.X, op=ALU.add), r=['tiny'], w=['tiny'])
        op('act', lambda e: e.activation(out=tiny[:, 96:98], in_=tiny[:, 96:98], func=AF.Exp), r=['tiny'], w=['tiny'])
        op('dve', lambda e: e.tensor_tensor(out=tiny[:, 98:99], in0=tiny[:, 96:97], in1=tiny[:, 97:98], op=ALU.subtract), r=['tiny'], w=['tiny'])
        op('dve', lambda e: e.tensor_scalar(out=sv[:, 840:841], in0=tiny[:, 98:99], scalar1=sv[:, SV_LAMC:SV_LAMC + 1], scalar2=-1.0, op0=ALU.add, op1=ALU.mult), r=['tiny', 'sv'], w=['sv'])
        op('dve', lambda e: e.tensor_scalar(out=sv[:, SV_DSUB:SV_DSUB + 96], in0=sv[:, SV_DSUB:SV_DSUB + 96], scalar1=sv[:, SV_LAMC + 1:SV_LAMC + 2], scalar2=None, op0=ALU.mult), r=['sv'], w=['sv'])
        neglam = sv[:, 840:841]

        P.mute = 'mod' not in PHASES
        m_mark = A.mark()
        wm = [A.alloc([128, 8, 512], BF16) for _ in range(2)]
        srep = A.alloc([128, 2, 8, 128], BF16)
        op('dve', lambda e: e.tensor_copy(out=srep, in_=bc(scol.unsqueeze(3), [128, 2, 8, 128])), r=['scol'], w=['srep'])
        bmb = A.alloc([128, 512], F32)
        ncol = A.alloc([128, 2, 8], F32)
        bcol = A.alloc([128, 48], F32)
        rowst = A.alloc([64, 128], F32)
        P.dma('sp', rowst[0:48, :], bmod_d[l].rearrange("(t p) -> t p", p=128), writes=['rowst'])
        P.dma('sp', rowst[48:56, :], n1_d[l].rearrange("(t p) -> t p", p=128), writes=['rowst'])
        P.dma('sp', rowst[56:64, :], n2_d[l].rearrange("(t p) -> t p", p=128), writes=['rowst'])
        mm(PB[6][:, 0:64], rowst, identf[0:64, 0:64], True, True, ['rowst', 'identf'], ['pb6'])
        op('dve', lambda e: e.tensor_copy(out=bcol, in_=PB[6][:, 0:48]), r=['pb6'], w=['bcol'])
        op('dve', lambda e: e.tensor_copy(out=ncol.rearrange("p a k -> p (a k)"), in_=PB[6][:, 48:64]), r=['pb6'], w=['ncol'])
        wmr = wmod_d[l].rearrange("(k p) n -> p k n", p=128)
        pcol = PB[5][:, 0:64].rearrange("p (c r) -> p c r", r=2)
        colidx = {0: 0, 1: 4, 2: 8, 3: 12, 6: 16, 7: 20, 8: 24, 9: 28}
        for j in range(12):
            wb_ = wm[j % 2]
            P.dma('pool', wb_, wmr[:, :, j * 512:(j + 1) * 512], writes=['wm%d' % (j % 2)])
            if j in colidx:
                for ct in range(4):
                    ci = colidx[j] + ct
                    for k in range(8):
                        mm(pcol[:, ci, :], wb_[:, k, ct * 128:(ct + 1) * 128], scol[:, :, k], k == 0, k == 7, ['wm%d' % (j % 2), 'scol'], ['pb5'])
            else:
                which = 0 if j < 6 else 1
                half = j % 2 if j < 6 else (j - 10)
                P.dma('sp', bmb, bmod_d[l, j * 512:(j + 1) * 512].partition_broadcast(128), writes=['bmb'])
                for r in range(2):
                    for k in range(8):
                        mm(PB[6], srep[:, r, k, :], wb_[:, k, :], k == 0, k == 7, ['wm%d' % (j % 2), 'srep'], ['pb6'])
                    op('dve', lambda e: e.tensor_tensor(out=gb[:, which, r, half * 512:(half + 1) * 512], in0=PB[6], in1=bmb, op=ALU.add), r=['pb6', 'bmb'], w=['gb'])
        for which, (sh_c, sc_c, sh_b, sc_b) in enumerate(((0, 8, 0, 8), (16, 24, 24, 32))):
            op('dve', lambda e: e.tensor_tensor(out=modB[:, which], in0=pcol[:, sh_c:sh_c + 8, :], in1=bc(bcol[:, sh_b:sh_b + 8].unsqueeze(2), [128, 8, 2]), op=ALU.add), r=['pb5', 'bcol'], w=['mod'])
            op('dve', lambda e: e.tensor_tensor(out=modA[:, which], in0=pcol[:, sc_c:sc_c + 8, :], in1=bc(bcol[:, sc_b:sc_b + 8].unsqueeze(2), [128, 8, 2]), op=ALU.add), r=['pb5', 'bcol', 'mod'], w=['mod'])
            op('dve', lambda e: e.tensor_scalar(out=modA[:, which], in0=modA[:, which], scalar1=1.0, scalar2=None, op0=ALU.add), r=['mod'], w=['mod'])
            op('dve', lambda e: e.tensor_tensor(out=modA[:, which], in0=modA[:, which], in1=bc(ncol[:, which, :].unsqueeze(2), [128, 8, 2]), op=ALU.mult), r=['mod', 'ncol'], w=['mod'])
        P.barrier()
        A.reset(m_mark)

        P.mute = 'A1' not in PHASES and 'A1a' not in PHASES
        a_mark = A.mark()
        RAWW = 2304 + 8
        raw = A.alloc([128, 4, RAWW], BF16)
        qkT = A.alloc([128, 4, NT * 128], BF16)
        ktok = A.alloc([128, NT, 256], BF16)
        Vm = A.alloc([128, NT, 4, 66], BF16)
        osig = A.alloc([128, NT, 256], BF16)
        gts = A.alloc([128, NT, 16], F32)
        lfm = A.alloc([128, NT, 8], F32)
        hbm = A.alloc([128, NT, 256], BF16)
        a1_mark = A.mark()
        DBG.update(raw=raw, qkT=qkT, ktok=ktok, Vm=Vm, osig=osig, gts=gts, lfm=lfm, hbm=hbm)
        hT = A.alloc([128, 8, 512], BF16)
        nscr = (A.alloc([128, D], BF16), A.alloc([128, D], BF16), A.alloc([128, 1], F32))
        wA = A.alloc([128, 8, 1040], BF16)
        sgs = A.alloc([128, 256], F32)
        print("SBUF A1 proj top", A.cur, A.top)

        winr = win_d[l].rearrange("(k p) n -> p k n", p=128)
        P.dma('pool', wA[:, :, 0:512], winr[:, :, 1792:2304], writes=['wA'])
        P.dma('pool', wA[:, :, 512:1040], winr[:, :, 2304:2832], writes=['wA'])
        op('pool', lambda e: e.memset(raw, 0.0), w=['raw'])
        op('pool', lambda e: e.memset(Vm, 1.0), w=['Vm'])

        def tokoff(t):
            return (2 + t * 128) if t < 16 else (2052 + (t - 16) * 128)

        for ci, tiles in enumerate(CHUNKS):
            ntok = len(tiles) * 128
            import os as _os
            _sk = set(_os.environ.get('DBGSKIP', '').split(','))
            _m0 = P.mute
            P.mute = _m0 or 'nT' in _sk
            norm_T(hT, tiles, 0, nscr)
            P.mute = _m0 or 'fm' in _sk
            for ct in range(4):
                pb_ = PB[ct % 2]
                for k in range(8):
                    mm(pb_[:, 0:ntok], wA[:, k, ct * 128:(ct + 1) * 128], hT[:, k, 0:ntok], k == 0, k == 7, ['wA', 'hT'], ['pb%d' % (ct % 2)])
                o0 = tokoff(tiles[0])
                op('act', lambda e: e.activation(out=raw[:, ct, o0:o0 + ntok], in_=pb_[:, 0:ntok], func=AF.Copy), r=['pb%d' % (ct % 2)], w=['raw'])
            P.mute = _m0 or 'tm' in _sk
            for i, t in enumerate(tiles):
                pb_ = PB[2 + i % 2]
                pk = 'pb%d' % (2 + i % 2)
                for k in range(8):
                    mm(pb_, hT[:, k, i * 128:(i + 1) * 128], wA[:, k, 512:1024], k == 0, k == 7, ['wA', 'hT'], [pk])
                P.mute = _m0 or 'tm' in _sk or 'vcopy' in _sk
                op('dve', lambda e: e.tensor_copy(out=Vm[:, t, :, 0:64], in_=pb_[:, 0:256].rearrange("p (h d) -> p h d", h=4)), r=[pk], w=['Vm'])
                P.mute = _m0 or 'tm' in _sk or 'sig' in _sk
                op('act', lambda e: e.activation(out=sgs, in_=pb_[:, 256:512], func=AF.Exp, scale=-1.0), r=[pk], w=['sgs'])
                op('dve', lambda e: e.tensor_scalar(out=sgs, in0=sgs, scalar1=1.0, scalar2=None, op0=ALU.add), r=['sgs'], w=['sgs'])
                op('dve', lambda e: e.reciprocal(out=sgs, in_=sgs), r=['sgs'], w=['sgs'])
                op('pool', lambda e: e.tensor_copy(out=osig[:, t, :], in_=sgs), r=['sgs'], w=['osig'])
                P.mute = _m0 or 'tm' in _sk or 'gates' in _sk
                for k in range(8):
                    mm(PB[4][:, 0:16], hT[:, k, i * 128:(i + 1) * 128], wA[:, k, 1024:1040], k == 0, k == 7, ['wA', 'hT'], ['pb4'])
                op('dve', lambda e: e.tensor_tensor(out=gts[:, t, :], in0=PB[4][:, 0:16], in1=sv[:, SV_GTB:SV_GTB + 16], op=ALU.add), r=['pb4', 'sv'], w=['gts'])
            P.mute = _m0
        P.barrier()
        A.reset(a1_mark)
        Cst = A.alloc([128, 2, 2, 66], F32)
        Cb = A.alloc([128, 2, 2, 66], BF16)
        WT = [A.alloc([128, 4, 128], BF16) for _ in range(2)]
        Vp = A.alloc([128, 4, 66], BF16)
        cscr = [A.alloc([128, 512], F32) for _ in range(2)]
        msm = A.alloc([128, 64], F32)
        Ctmp = A.alloc([128, 2, 66], F32)
        hsc = [A.alloc([128, 256], F32) for _ in range(2)]
        P.mute = 'A1' not in PHASES and 'A1b' not in PHASES
        op('pool', lambda e: e.memset(Cst, 0.0), w=['Cst'])
        op('pool', lambda e: e.memset(Cb, 0.0), w=['Cb'])
        g4 = gts.rearrange("p t (a h) -> p t a h", a=4)
        l4 = lfm.rearrange("p t (a h) -> p t a h", a=2)
        op('act', lambda e: e.activation(out=l4, in_=g4[:, :, 1:4:2, :], func=AF.Exp, scale=-1.0), r=['gts'], w=['lfm'])
        op('act', lambda e: e.activation(out=l4, in_=l4, func=AF.Ln, bias=kc[:, 2:3]), r=['lfm'], w=['lfm'])
        op('dve', lambda e: e.tensor_scalar(out=l4, in0=l4, scalar1=-1.0, scalar2=None, op0=ALU.mult), r=['lfm'], w=['lfm'])
        for ct in range(4):
            for (o0, n) in ((2, 512), (514, 512), (1026, 512), (1538, 512), (2052, 256)):
                s0, s1 = cscr
                dst0 = o0 - 2 if o0 < 2052 else 2048
                op('pool', lambda e: e.tensor_scalar(out=s0[:, 0:n], in0=raw[:, ct, o0 - 1:o0 - 1 + n], scalar1=cwc[:, 0, ct:ct + 1], scalar2=None, op0=ALU.mult), r=['raw', 'cwc'], w=['cs0'])
                op('dve', lambda e: e.scalar_tensor_tensor(out=s1[:, 0:n], in0=raw[:, ct, o0:o0 + n], scalar=cwc[:, 1, ct:ct + 1], in1=s0[:, 0:n], op0=ALU.mult, op1=ALU.add), r=['raw', 'cwc', 'cs0'], w=['cs1'])
                op('dve', lambda e: e.scalar_tensor_tensor(out=s0[:, 0:n], in0=raw[:, ct, o0 + 1:o0 + 1 + n], scalar=cwc[:, 2, ct:ct + 1], in1=s1[:, 0:n], op0=ALU.mult, op1=ALU.add), r=['raw', 'cwc', 'cs1', 'cs0'], w=['cs0'])
                op('act', lambda e: e.activation(out=qkT[:, ct, dst0:dst0 + n], in_=s0[:, 0:n], func=AF.Silu, bias=cbc[:, ct:ct + 1]), r=['cs0', 'cwc'], w=['qkT'])
        for t in range(NT):
            for pr in range(2):
                pst = PBH[pr][:, 0:128]
                op('pe', lambda e: e.transpose(pst, qkT[:, 2 + pr, t * 128:(t + 1) * 128], identb), r=['qkT', 'identb'], w=['pb%d' % pr])
                op('dve' if pr == 0 else 'act', (lambda e: e.tensor_copy(out=ktok[:, t, pr * 128:(pr + 1) * 128], in_=pst)) if pr == 0 else
                   (lambda e: e.activation(out=ktok[:, t, pr * 128:(pr + 1) * 128], in_=pst, func=AF.Copy)), r=['pb%d' % pr], w=['ktok'])
        P.mute = 'A1' not in PHASES and 'A1c' not in PHASES
        for di in (1, 0):
            tri = trif if di == 0 else trib
            trik = 'trif' if di == 0 else 'trib'
            order = [16, 17] + list(range(16)) if di == 0 else [17, 16] + list(range(15, -1, -1))
            for t in order:
                tcols = slice(t * 128, (t + 1) * 128)
                lf = lfm[:, t, di * 4:di * 4 + 4]
                li = gts[:, t, di * 8:di * 8 + 4]
                mm(PB[0][:, 0:4], tri, lf, True, True, [trik, 'lfm'], ['pb0'])
                mm(PB[0][:, 8:12], ones64, lf, True, True, ['ones64', 'lfm'], ['pb0'])
                cs_ = msm[:, 0:4]
                eb_ = msm[:, 4:8]
                eB_ = msm[:, 8:12]
                op('dve', lambda e: e.tensor_tensor(out=cs_, in0=li, in1=PB[0][:, 0:4], op=ALU.subtract), r=['gts', 'pb0'], w=['m_cs'])
                op('act', lambda e: e.activation(out=cs_, in_=cs_, func=AF.Exp, bias=ln8_ap), r=['m_cs'], w=['m_cs'])
                op('act', lambda e: e.activation(out=eb_, in_=PB[0][:, 0:4], func=AF.Exp), r=['pb0'], w=['m_eb'])
                op('act', lambda e: e.activation(out=eB_, in_=PB[0][:, 8:12], func=AF.Exp), r=['pb0'], w=['m_eB'])
                wt = WT[t % 2]
                wk = 'WT%d' % (t % 2)
                ps_e = PB[1].rearrange("p (h n) -> p h n", h=4)
                ps_o = PB[4].rearrange("p (h n) -> p h n", h=4)
                for h in range(4):
                    pb0 = (h % 2) * 64
                    pss = ps_e if h % 2 == 0 else ps_o
                    mm(pss[:, h // 2, :], qkT[pb0:pb0 + 64, 2 + h // 2, tcols], qkT[pb0:pb0 + 64, h // 2, tcols], True, True, ['qkT'], ['pb1' if h % 2 == 0 else 'pb4'])
                for h in range(4):
                    pss = ps_e if h % 2 == 0 else ps_o
                    op('dve', lambda e: e.scalar_tensor_tensor(out=wt[:, h, :], in0=pss[:, h // 2, :], scalar=cs_[:, h:h + 1], in1=tri, op0=ALU.mult, op1=ALU.mult), r=['pb1' if h % 2 == 0 else 'pb4', 'm_cs', trik], w=[wk])
                op('pool', lambda e: e.tensor_tensor(out=Vp, in0=Vm[:, t], in1=bc(cs_.unsqueeze(2), [128, 4, 66]), op=ALU.mult), r=['Vm', 'm_cs'], w=['Vp'])
                pso = PB[2][:, 0:260].rearrange("p (h n) -> p h n", h=4)
                for h in range(4):
                    pb0 = (h % 2) * 64
                    mm(pso[:, h, :], wt[:, h, :], Vm[:, t, h, 0:65], True, False, [wk, 'Vm'], ['pb2'])
                    mm(pso[:, h, :], qkT[pb0:pb0 + 64, h // 2, tcols], Cb[pb0:pb0 + 64, di, h // 2, 0:65], False, True, ['qkT', 'Cb'], ['pb2'])
                psu = PB[3][:, 0:260].rearrange("p (h n) -> p h n", h=4)
                for h in range(4):
                    mm(psu[:, h, :], ktok[:, t, (h // 2) * 128:(h // 2 + 1) * 128], Vp[:, h, 0:65], True, True, ['ktok', 'Vp'], ['pb3'])
                for hh in range(2):
                    ps_ = slice(hh * 64, hh * 64 + 64)
                    op('dve', lambda e: e.tensor_tensor(out=Ctmp[ps_, :, 0:65], in0=psu[ps_, hh:4:2, :], in1=Cst[ps_, di, :, 0:65], op=ALU.add), r=['pb3', 'Cst'], w=['Ctmp'])
                    op('dve', lambda e: e.tensor_tensor(out=Cst[ps_, di, :, 0:65], in0=Ctmp[ps_, :, 0:65], in1=bc(eB_[ps_, hh:4:2].unsqueeze(2), [64, 2, 65]), op=ALU.mult), r=['Ctmp', 'm_eB'], w=['Cst'])
                op('pool', lambda e: e.tensor_copy(out=Cb[:, di], in_=Cst[:, di]), r=['Cst'], w=['Cb'])
                den = msm[:, 12:16]
                nd = msm[:, 16:20]
                op('dve', lambda e: e.tensor_tensor(out=den, in0=pso[:, :, 64], in1=eb_, op=ALU.mult), r=['pb2', 'm_eb'], w=['m_den'])
                op('dve', lambda e: e.tensor_scalar(out=nd, in0=den, scalar1=-1.0, scalar2=None, op0=ALU.mult), r=['m_den'], w=['m_nd'])
                op('dve', lambda e: e.tensor_tensor(out=den, in0=den, in1=nd, op=ALU.max), r=['m_den', 'm_nd'], w=['m_den'])
                op('dve', lambda e: e.tensor_scalar(out=den, in0=den, scalar1=1.0, scalar2=None, op0=ALU.max), r=['m_den'], w=['m_den'])
                op('dve', lambda e: e.reciprocal(out=den, in_=den), r=['m_den'], w=['m_den'])
                op('dve', lambda e: e.tensor_tensor(out=den, in0=den, in1=eb_, op=ALU.mult), r=['m_den', 'm_eb'], w=['m_den'])
                if di == 1:
                    op('dve', lambda e: e.tensor_tensor(out=hbm[:, t, :].rearrange("p (h d) -> p h d", h=4), in0=pso[:, :, 0:64], in1=bc(den.unsqueeze(2), [128, 4, 64]), op=ALU.mult), r=['pb2', 'm_den'], w=['hbm%d' % t])
                else:
                    h0, h1 = hsc
                    h0v = h0.rearrange("p (h d) -> p h d", h=4)
                    h1v = h1.rearrange("p (h d) -> p h d", h=4)
                    ssn = msm[:, 20:24]
                    op('dve', lambda e: e.tensor_tensor(out=h0v, in0=pso[:, :, 0:64], in1=bc(den.unsqueeze(2), [128, 4, 64]), op=ALU.mult), r=['pb2', 'm_den'], w=['h0'])
                    op('pool', lambda e: e.tensor_tensor(out=h0, in0=h0, in1=hbm[:, t, :], op=ALU.add), r=['h0', 'hbm%d' % t], w=['h0'])
                    op('pool', lambda e: e.tensor_tensor(out=h1, in0=h0, in1=h0, op=ALU.mult), r=['h0'], w=['h1'])
                    op('dve', lambda e: e.tensor_reduce(out=ssn, in_=h1v, axis=AX.X, op=ALU.add), r=['h1'], w=['m_ssn'])
                    rstd_from_ss(ssn, 4, 64, ['m_ssn'], ['m_ssn'])
                    op('dve', lambda e: e.tensor_tensor(out=h1v, in0=h0v, in1=bc(ssn.unsqueeze(2), [128, 4, 64]), op=ALU.mult), r=['h0', 'm_ssn', 'h1'], w=['h1'])
                    op('pool', lambda e: e.tensor_tensor(out=h1, in0=h1, in1=sv[:, SV_HNG:SV_HNG + 256], op=ALU.mult), r=['h1', 'sv'], w=['h1'])
                    op('pool', lambda e: e.tensor_tensor(out=hbm[:, t, :], in0=h1, in1=osig[:, t, :], op=ALU.mult), r=['h1', 'osig', 'hbm%d' % t], w=['hbm%d' % t])
                    P.dma('sp', hmd_d[t * 128:(t + 1) * 128, :], hbm[:, t, :], reads=['hbm%d' % t], writes=['hmd%d' % t])
        P.barrier()
        A.reset(a_mark)

        P.mute = 'A2' not in PHASES
        hT = A.alloc([128, 8, 512], BF16)
        wq = A.alloc([128, 8, 768], BF16)
        wkv = A.alloc([128, 8, 1024], BF16)
        dkT = A.alloc([128, 4, NT * 128], BF16)
        gkT = A.alloc([128, NT * 128], BF16)
        Vd = A.alloc([128, NT, 4, 98], BF16)
        Vg = A.alloc([128, NT, 2, 66], BF16)
        dqT = A.alloc([128, 4, 512], BF16)
        gqT = A.alloc([128, 3, 512], BF16)
        Eb = [A.alloc([128, 512], BF16) for _ in range(3)]
        pad = [A.alloc([128, 8, 64], BF16) for _ in range(2)]
        mix = A.alloc([128, 4, D], BF16)
        mixT = A.alloc([128, 8, 128], BF16)
        qs = (A.alloc([128, 384], F32), A.alloc([128, 384], F32), A.alloc([128, 384], F32), A.alloc([128, 8], F32))
        osA = A.alloc([128, 4, 96], F32)
        osB = A.alloc([128, 4, 96], F32)
        ysc = A.alloc([128, 512], F32)
        nscr = (ysc.bitcast(BF16), A.alloc([128, D], BF16), A.alloc([128, 1], F32), 'ysc')
        ar = A.alloc([128, 8], F32)
        DBG.update(dkT=dkT, gkT=gkT, Vd=Vd, Vg=Vg, dqT=dqT, gqT=gqT, mix=mix)
        print('SBUF A2 top', A.cur, A.top)

        P.dma('pool', wq[:, :, 0:384], winr[:, :, 0:384], writes=['wq'])
        P.dma('pool', wq[:, :, 384:768], winr[:, :, 1152:1536], writes=['wq'])
        P.dma('pool', wkv[:, :, 0:768], winr[:, :, 384:1152], writes=['wkv'])
        P.dma('pool', wkv[:, :, 768:1024], winr[:, :, 1536:1792], writes=['wkv'])
        op('pool', lambda e: e.memset(Vd, 1.0), w=['Vd'])
        op('pool', lambda e: e.memset(Vg, 1.0), w=['Vg'])
        for p_ in pad:
            op('pool', lambda e: e.memset(p_, 0.0), w=['pad0', 'pad1'])

        def to_T(pad_ap, ncolt, dst_fn, psb, psk, pk):
            for j in range(ncolt):
                op('pe', lambda e: e.transpose(PBH[psb][:, j * 128:(j + 1) * 128], pad_ap[:, j * 128:(j + 1) * 128], identb), r=[pk, 'identb'], w=[psk])
            for j in range(ncolt):
                if psb % 2 == 0:
                    op('dve', lambda e: e.tensor_copy(out=dst_fn(j), in_=PBH[psb][:, j * 128:(j + 1) * 128]), r=[psk], w=['T_dst'])
                else:
                    op('act', lambda e: e.activation(out=dst_fn(j), in_=PBH[psb][:, j * 128:(j + 1) * 128], func=AF.Copy), r=[psk], w=['T_dst'])

        for ci, tiles in enumerate(CHUNKS):
            norm_T(hT, tiles, 0, nscr)
            for i, t in enumerate(tiles):
                hs = slice(i * 128, (i + 1) * 128)
                for k in range(8):
                    mm(PB[0][:, 0:384], hT[:, k, hs], wkv[:, k, 0:384], k == 0, k == 7, ['wkv', 'hT'], ['pb0'])
                qknr(PB[0][:, 0:384], 8, 48, sv[:, SV_DKN:SV_DKN + 48], cos48[:, t, :], sin48[:, t, :], pad[0][:, :, 0:48], qs, 'pb0')
                to_T(pad[0].rearrange("p u d -> p (u d)"), 4, lambda j: dkT[:, j, t * 128:(t + 1) * 128], 4, 'pb4', 'q_out')
                for k in range(8):
                    mm(PB[1][:, 0:384], hT[:, k, hs], wkv[:, k, 384:768], k == 0, k == 7, ['wkv', 'hT'], ['pb1'])
                op('act', lambda e: e.activation(out=Vd[:, t, :, 0:96], in_=PB[1][:, 0:384].rearrange("p (h d) -> p h d", h=4), func=AF.Copy), r=['pb1'], w=['Vd'])
                for k in range(8):
                    mm(PB[2][:, 0:256], hT[:, k, hs], wkv[:, k, 768:1024], k == 0, k == 7, ['wkv', 'hT'], ['pb2'])
                qknr(PB[2][:, 0:128], 2, 64, sv[:, SV_GKN:SV_GKN + 64], cos64[:, t, :], sin64[:, t, :], pad[1][:, 0:2, :], qs, 'pb2')
                to_T(pad[1].rearrange("p u d -> p (u d)"), 1, lambda j: gkT[:, t * 128:(t + 1) * 128], 5, 'pb5', 'q_out')
                op('act', lambda e: e.activation(out=Vg[:, t, :, 0:64], in_=PB[2][:, 128:256].rearrange("p (h d) -> p h d", h=2), func=AF.Copy), r=['pb2'], w=['Vg'])
        woutr = wout_d[l].rearrange("(k p) n -> p k n", p=128)
        P.dma('pool', wkv, woutr, reads=['wkv'], writes=['wkv'])

        sc48 = 48.0 ** -0.5
        sc64 = 0.125
        qchunks = CHUNKS if upd_ctx else CHUNKS[:4]
        ei = 0
        for ci, tiles in enumerate(qchunks):
            nq = len(tiles)
            nqt = nq * 128
            r_ = 0 if tiles[0] < 16 else 1
            ktiles = list(range(NT)) if r_ == 0 else [16, 17]
            norm_T(hT, tiles, 0, nscr)
            for i, t in enumerate(tiles):
                hs = slice(i * 128, (i + 1) * 128)
                for k in range(8):
                    mm(PB[0][:, 0:384], hT[:, k, hs], wq[:, k, 0:384], k == 0, k == 7, ['wq', 'hT'], ['pb0'])
                qknr(PB[0][:, 0:384], 8, 48, sv[:, SV_DQN:SV_DQN + 48], cos48[:, t, :], sin48[:, t, :], pad[0][:, :, 0:48], qs, 'pb0')
                to_T(pad[0].rearrange("p u d -> p (u d)"), 4, lambda j: dqT[:, j, hs], 4, 'pb4', 'q_out')
                for k in range(8):
                    mm(PB[1][:, 0:384], hT[:, k, hs], wq[:, k, 384:768], k == 0, k == 7, ['wq', 'hT'], ['pb1'])
                qknr(PB[1][:, 0:384], 6, 64, sv[:, SV_GQN:SV_GQN + 64], cos64[:, t, :], sin64[:, t, :], pad[1][:, 0:6, :].rearrange("p (j k) d -> p k j d", k=2), qs, 'pb1', split=2)
                to_T(pad[1].rearrange("p u d -> p (u d)"), 3, lambda j: gqT[:, j, hs], 5, 'pb5', 'q_out')
                P.dma('sp', mix[:, i, 768:1024], hmd_d[t * 128:(t + 1) * 128, :], reads=['hmd%d' % t], writes=['mix'])
            items = []
            nkt = len(ktiles)

            def diff_post(h, b0, b1):
                def f():
                    po0 = PB[b0][:, 0:nq * 97].rearrange("p (q n) -> p q n", q=nq)
                    po1 = PB[b1][:, 0:nq * 97].rearrange("p (q n) -> p q n", q=nq)
                    k0, k1 = 'pb%d' % b0, 'pb%d' % b1
                    r1 = ar[:, 0:nq]
                    r2 = ar[:, 4:4 + nq]
                    oa = osA[:, 0:nq, :]
                    ob = osB[:, 0:nq, :]
                    op('dve', lambda e: e.reciprocal(out=r1, in_=po0[:, :, 96]), r=[k0], w=['a_r1'])
                    op('dve', lambda e: e.reciprocal(out=r2, in_=po1[:, :, 96]), r=[k1], w=['a_r2'])
                    op('dve', lambda e: e.tensor_scalar(out=r2, in0=r2, scalar1=neglam, scalar2=None, op0=ALU.mult), r=['a_r2', 'sv'], w=['a_r2'])
                    op('dve', lambda e: e.tensor_tensor(out=oa, in0=po0[:, :, 0:96], in1=bc(r1.unsqueeze(2), [128, nq, 96]), op=ALU.mult), r=[k0, 'a_r1'], w=['osA'])
                    op('dve', lambda e: e.tensor_tensor(out=ob, in0=po1[:, :, 0:96], in1=bc(r2.unsqueeze(2), [128, nq, 96]), op=ALU.mult), r=[k1, 'a_r2'], w=['osB'])
                    op('pool', lambda e: e.tensor_tensor(out=oa, in0=oa, in1=ob, op=ALU.add), r=['osA', 'osB'], w=['osA'])
                    op('pool', lambda e: e.tensor_tensor(out=ob, in0=oa, in1=oa, op=ALU.mult), r=['osA', 'osB'], w=['osB'])
                    op('dve', lambda e: e.tensor_reduce(out=r1, in_=ob, axis=AX.X, op=ALU.add), r=['osB', 'a_r1'], w=['a_r1'])
                    rstd_from_ss(r1, nq, 96, ['a_r1'], ['a_r1'])
                    op('dve', lambda e: e.tensor_tensor(out=ob, in0=oa, in1=bc(r1.unsqueeze(2), [128, nq, 96]), op=ALU.mult), r=['osA', 'a_r1', 'osB'], w=['osB'])
                    op('pool', lambda e: e.tensor_tensor(out=mix[:, 0:nq, h * 96:(h + 1) * 96], in0=ob, in1=bc(sv[:, SV_DSUB:SV_DSUB + 96].unsqueeze(1), [128, nq, 96]), op=ALU.mult), r=['osB', 'sv'], w=['mix'])
                return f

            def gqa_post(g, bk):
                def f():
                    pso = PB[bk][:, 0:nq * 65].rearrange("p (q n) -> p q n", q=nq)
                    pok = 'pb%d' % bk
                    rg = ar[:, 0:nq] if g % 2 == 0 else ar[:, 4:4 + nq]
                    rgk = 'a_r1' if g % 2 == 0 else 'a_r2'
                    op('dve', lambda e: e.reciprocal(out=rg, in_=pso[:, :, 64]), r=[pok], w=[rgk])
                    op('dve', lambda e: e.tensor_tensor(out=mix[:, 0:nq, 384 + g * 64:384 + (g + 1) * 64], in0=pso[:, :, 0:64], in1=bc(rg.unsqueeze(2), [128, nq, 64]), op=ALU.mult), r=[pok, rgk], w=['mix'])
                return f

            for h in range(4):
                banks = (2, 3) if h % 2 == 0 else (4, 5)
                for half in range(2):
                    u = 2 * h + half
                    for kidx, kt in enumerate(ktiles):
                        post = diff_post(h, banks[0], banks[1]) if (half == 1 and kidx == nkt - 1) else None
                        items.append(dict(kind='d', lhs=dkT[(u % 2) * 64:(u % 2) * 64 + 48, u // 2, kt * 128:(kt + 1) * 128],
                                          rhs=dqT[(u % 2) * 64:(u % 2) * 64 + 48, u // 2, 0:nqt], sc=sc48,
                                          V=Vd[:, kt, h, 0:97], vk='Vd', W=97, bank=banks[half], first=(kidx == 0), last=(kidx == nkt - 1), post=post))
            for g in range(6):
                kvh = g // 3
                bk = 2 + g % 4
                for kidx, kt in enumerate(ktiles):
                    post = gqa_post(g, bk) if kidx == nkt - 1 else None
                    items.append(dict(kind='g', lhs=gkT[kvh * 64:(kvh + 1) * 64, kt * 128:(kt + 1) * 128],
                                      rhs=gqT[kvh * 64:(kvh + 1) * 64, g % 3, 0:nqt], sc=sc64,
                                      V=Vg[:, kt, kvh, 0:65], vk='Vg', W=65, bank=bk, first=(kidx == 0), last=(kidx == nkt - 1), post=post))

            def emit_S(i):
                it = items[i]
                sb_ = i % 2
                mm(PB[sb_][:, 0:nqt], it['lhs'], it['rhs'], True, True, ['T_dst'], ['pb%d' % sb_])

            def emit_EPV(i):
                it = items[i]
                sb_ = i % 2
                eb_i = i % 3
                op('act', lambda e: e.activation(out=Eb[eb_i][:, 0:nqt], in_=PB[sb_][:, 0:nqt], func=AF.Exp, scale=it['sc']), r=['pb%d' % sb_], w=['E%d' % eb_i])
                pso = PB[it['bank']][:, 0:nq * it['W']].rearrange("p (q n) -> p q n", q=nq)
                for q in range(nq):
                    mm(pso[:, q, :], Eb[eb_i][:, q * 128:(q + 1) * 128], it['V'], it['first'] and q == 0, it['last'], ['E%d' % eb_i, it['vk']], ['pb%d' % it['bank']], skip=True)
                if it['post'] is not None:
                    it['post']()

            emit_S(0)
            for i in range(len(items)):
                if i + 1 < len(items):
                    emit_S(i + 1)
                emit_EPV(i)
            for i, t in enumerate(tiles):
                pst = PBH[6].rearrange("p (k n) -> p k n", k=8)
                for k in range(8):
                    op('pe', lambda e: e.transpose(pst[:, k, :], mix[:, i, k * 128:(k + 1) * 128], identb), r=['mix', 'identb'], w=['pb6'])
                op('act', lambda e: e.activation(out=mixT, in_=pst, func=AF.Copy), r=['pb6'], w=['mixT'])
                for n in range(2):
                    ns = slice(n * 512, (n + 1) * 512)
                    for k in range(8):
                        mm(PB[7], mixT[:, k, :], wkv[:, k, ns], k == 0, k == 7, ['mixT', 'wkv'], ['pb7'])
                    op('dve', lambda e: e.tensor_tensor(out=ysc, in0=PB[7], in1=gb[:, 0, r_, ns], op=ALU.mult), r=['pb7', 'gb'], w=['ysc'])
                    op('pool', lambda e: e.tensor_tensor(out=xs[:, t, ns], in0=xs[:, t, ns], in1=ysc, op=ALU.add), r=['ysc', 'xs%d' % t], w=['xs%d' % t])
        P.barrier()
        A.reset(a_mark)

        P.mute = 'B' not in PHASES
        act_tiles = list(range(NT)) if upd_ctx else list(range(16))
        NTa = len(act_tiles)
        NB = NTa * 2 + NE
        ftok = A.alloc([128, NT, D], BF16)
        slot_i = A.alloc([128, NT, 2], I32)
        gate2 = A.alloc([128, NT, 2], F32)
        blkexp_i = A.alloc([128, 104], I32)
        idx1 = A.alloc([128, 104, 8], I32)
        idx2 = A.alloc([128, 104, 4], I32)
        b1_mark = A.mark()
        modrow = A.alloc([128, 2, 2, D], F32)
        wm_mark = A.mark()
        wm2 = [A.alloc([128, 8, 512], BF16) for _ in range(2)]
        bmb2 = A.alloc([128, 512], F32)
        n2row = A.alloc([128, D], F32)
        srep = A.alloc([128, 2, 8, 128], BF16)
        fsc = A.alloc([128, D], F32)
        junkb = A.alloc([128, D], BF16)
        fTt = A.alloc([128, 8, 128], BF16)
        wr = A.alloc([128, 8, 36], BF16)
        rsc = A.alloc([128, 160], F32)
        ssb = A.alloc([128, 1], F32)
        Mb = A.alloc([128, NT, 32], BF16)
        M1 = A.alloc([128, NT, 32], F32)
        M2 = A.alloc([128, NT, 32], F32)
        rank = A.alloc([128, NT, 32], F32)
        Mcum = A.alloc([128, 32], BF16)
        ev = A.alloc([128, 6, 32], F32)
        slotf = A.alloc([128, NT, 2], F32)
        _sv = A.mark()
        A.reset(wm_mark)
        cmpb = A.alloc([128, 104, 32], F32)
        A.reset(_sv)
        bef = A.alloc([128, 104], F32)
        idxf1 = A.alloc([128, 104, 8], F32)
        idxf2 = A.alloc([128, 104, 4], F32)
        print('SBUF B1 top', A.cur, A.top)
        op('dve', lambda e: e.tensor_copy(out=srep, in_=bc(scol.unsqueeze(3), [128, 2, 8, 128])), r=['scol'], w=['srep'])
        P.dma('sp', n2row, n2_d[l].partition_broadcast(128), writes=['n2row'])
        P.dma('pool', wr[:, :, 0:4], wg_d[l].rearrange("(k p) n -> p k n", p=128), writes=['wr'])
        P.dma('pool', wr[:, :, 4:36], we_d[l].rearrange("(k p) n -> p k n", p=128), writes=['wr'])
        for j in (6, 7, 8, 9):
            wb_ = wm2[j % 2]
            ab = 1 if j < 8 else 0
            half = j % 2
            P.dma('pool', wb_, wmr[:, :, j * 512:(j + 1) * 512], writes=['wm%d' % (j % 2)])
            P.dma('sp', bmb2, bmod_d[l, j * 512:(j + 1) * 512].partition_broadcast(128), writes=['bmb'])
            for r in range(2):
                for k in range(8):
                    mm(PB[6], srep[:, r, k, :], wb_[:, k, :], k == 0, k == 7, ['wm%d' % (j % 2), 'srep'], ['pb6'])
                op('dve', lambda e: e.tensor_tensor(out=modrow[:, ab, r, half * 512:(half + 1) * 512], in0=PB[6], in1=bmb2, op=ALU.add), r=['pb6', 'bmb'], w=['modrow'])
        for r in range(2):
            op('dve', lambda e: e.scalar_tensor_tensor(out=modrow[:, 0, r, :], in0=modrow[:, 0, r, :], scalar=1.0, in1=n2row, op0=ALU.add, op1=ALU.mult), r=['modrow', 'n2row'], w=['modrow'])
        for t in act_tiles:
            r_ = 0 if t < 16 else 1
            op('pool', lambda e: e.memset(ssb, 0.0), w=['ssb'])
            op('act', lambda e: e.activation(out=junkb, in_=xs[:, t, :], func=AF.Square, accum_out=ssb), r=['xs%d' % t, 'ssb'], w=['junkb', 'ssb'])
            rstd_from_ss(ssb, 1, D, ['ssb'], ['ssb'])
            op('dve', lambda e: e.scalar_tensor_tensor(out=fsc, in0=xs[:, t, :], scalar=ssb, in1=modrow[:, 0, r_, :], op0=ALU.mult, op1=ALU.mult), r=['xs%d' % t, 'ssb', 'modrow'], w=['fsc'])
            op('pool', lambda e: e.tensor_tensor(out=ftok[:, t, :], in0=fsc, in1=modrow[:, 1, r_, :], op=ALU.add), r=['fsc', 'modrow'], w=['ftok%d' % t])
            pst = PBH[7].rearrange("p (k n) -> p k n", k=8)
            for k in range(8):
                op('pe', lambda e: e.transpose(pst[:, k, :], ftok[:, t, k * 128:(k + 1) * 128], identb), r=['ftok%d' % t, 'identb'], w=['pb7'])
            op('act', lambda e: e.activation(out=fTt, in_=pst, func=AF.Copy), r=['pb7'], w=['fTt'])
            for k in range(8):
                mm(PB[0][:, 0:36], fTt[:, k, :], wr[:, k, :], k == 0, k == 7, ['fTt', 'wr'], ['pb0'])
            lg = rsc[:, 0:36]
            gmax = rsc[:, 36:37]
            ngmax = rsc[:, 37:38]
            gsum = rsc[:, 38:39]
            eg = rsc[:, 40:44]
            maskg = rsc[:, 44:48]
            em = rsc[:, 48:80]
            m1 = rsc[:, 80:81]
            em2 = rsc[:, 128:160]
            m2 = rsc[:, 81:82]
            dd = rsc[:, 82:83]
            p1 = gate2[:, t, 0:1]
            p2 = gate2[:, t, 1:2]
            mask1 = M1[:, t, :]
            mask2 = M2[:, t, :]
            rk = ['rsc']
            op('dve', lambda e: e.tensor_tensor(out=lg, in0=PB[0][:, 0:36], in1=sv[:, SV_RB:SV_RB + 36], op=ALU.add), r=['pb0', 'sv', 'rsc'], w=rk)
            op('dve', lambda e: e.tensor_reduce(out=gmax, in_=lg[:, 0:4], axis=AX.X, op=ALU.max), r=rk, w=rk)
            op('dve', lambda e: e.tensor_scalar(out=ngmax, in0=gmax, scalar1=-1.0, scalar2=None, op0=ALU.mult), r=rk, w=rk)
            op('pool', lambda e: e.memset(gsum, 0.0), r=rk, w=rk)
            op('act', lambda e: e.activation(out=eg, in_=lg[:, 0:4], func=AF.Exp, bias=ngmax, accum_out=gsum), r=rk, w=rk)
            op('dve', lambda e: e.reciprocal(out=gsum, in_=gsum), r=rk, w=rk)
            op('dve', lambda e: e.tensor_scalar(out=maskg, in0=lg[:, 0:4], scalar1=gmax, scalar2=None, op0=ALU.is_equal), r=rk, w=rk)
            op('dve', lambda e: e.tensor_scalar(out=maskg, in0=maskg, scalar1=-1.0, scalar2=1e30, op0=ALU.add, op1=ALU.mult), r=rk, w=rk)
            op('dve', lambda e: e.tensor_tensor(out=em.rearrange("p (g x) -> p g x", g=4), in0=lg[:, 4:36].rearrange("p (g x) -> p g x", g=4), in1=bc(maskg.unsqueeze(2), [128, 4, 8]), op=ALU.add), r=rk, w=rk)
            op('dve', lambda e: e.tensor_reduce(out=m1, in_=em, axis=AX.X, op=ALU.max), r=rk, w=rk)
            op('dve', lambda e: e.tensor_scalar(out=mask1, in0=em, scalar1=m1, scalar2=None, op0=ALU.is_equal), r=rk, w=rk + ['M'])
            op('dve', lambda e: e.scalar_tensor_tensor(out=em2, in0=mask1, scalar=-1e30, in1=em, op0=ALU.mult, op1=ALU.add), r=rk + ['M'], w=rk)
            op('dve', lambda e: e.tensor_reduce(out=m2, in_=em2, axis=AX.X, op=ALU.max), r=rk, w=rk)
            op('dve', lambda e: e.tensor_scalar(out=mask2, in0=em2, scalar1=m2, scalar2=None, op0=ALU.is_equal), r=rk, w=rk + ['M'])
            op('pool', lambda e: e.tensor_tensor(out=Mb[:, t, :], in0=mask1, in1=mask2, op=ALU.add), r=['M'], w=['Mb'])
            op('dve', lambda e: e.tensor_tensor(out=dd, in0=m2, in1=m1, op=ALU.subtract), r=rk, w=rk)
            op('act', lambda e: e.activation(out=dd, in_=dd, func=AF.Exp), r=rk, w=rk)
            op('dve', lambda e: e.tensor_scalar(out=p1, in0=dd, scalar1=1.0, scalar2=None, op0=ALU.add), r=rk, w=rk + ['gate2'])
            op('dve', lambda e: e.reciprocal(out=p1, in_=p1), r=['gate2'], w=['gate2'])
            op('dve', lambda e: e.tensor_tensor(out=p1, in0=p1, in1=gsum, op=ALU.mult), r=rk + ['gate2'], w=['gate2'])
            op('dve', lambda e: e.tensor_tensor(out=p2, in0=p1, in1=dd, op=ALU.mult), r=rk + ['gate2'], w=['gate2'])
        op('pool', lambda e: e.memset(Mcum, 0.0), w=['Mcum'])
        for ti, t in enumerate(act_tiles):
            pbk = 1 + ti % 2
            mm(PB[pbk][:, 0:32], trisb, Mb[:, t, :], True, False, ['trisb', 'Mb'], ['pb%d' % pbk])
            mm(PB[pbk][:, 0:32], onesb, Mcum, False, True, ['onesb', 'Mcum'], ['pb%d' % pbk])
            op('dve', lambda e: e.tensor_copy(out=rank[:, t, :], in_=PB[pbk][:, 0:32]), r=['pb%d' % pbk], w=['rank'])
            op('pool', lambda e: e.tensor_tensor(out=Mcum, in0=Mcum, in1=Mb[:, t, :], op=ALU.add), r=['Mcum', 'Mb'], w=['Mcum'])
        mm(PB[3][:, 0:32], onesb, Mcum, True, True, ['onesb', 'Mcum'], ['pb3'])
        cnt, nblk, scA, scB, pend, pstart = [ev[:, i, :] for i in range(6)]
        op('dve', lambda e: e.tensor_copy(out=cnt, in_=PB[3][:, 0:32]), r=['pb3'], w=['ev'])
        op('dve', lambda e: e.memset(nblk, 0.0), r=['ev'], w=['ev'])
        for j in range(NT):
            op('dve', lambda e: e.scalar_tensor_tensor(out=nblk, in0=cnt, scalar=128.0 * j, in1=nblk, op0=ALU.is_gt, op1=ALU.add), r=['ev'], w=['ev'])
        op('dve', lambda e: e.tensor_copy(out=scA, in_=nblk), r=['ev'], w=['ev'])
        src_, dst_ = scA, scB
        for sft in (1, 2, 4, 8, 16):
            op('dve', lambda e: e.tensor_copy(out=dst_, in_=src_), r=['ev'], w=['ev'])
            op('dve', lambda e: e.tensor_tensor(out=dst_[:, sft:32], in0=src_[:, sft:32], in1=src_[:, 0:32 - sft], op=ALU.add), r=['ev'], w=['ev'])
            src_, dst_ = dst_, src_
        op('dve', lambda e: e.tensor_scalar(out=pend, in0=src_, scalar1=128.0, scalar2=None, op0=ALU.mult), r=['ev'], w=['ev'])
        op('dve', lambda e: e.scalar_tensor_tensor(out=pstart, in0=nblk, scalar=-128.0, in1=pend, op0=ALU.mult, op1=ALU.add), r=['ev'], w=['ev'])
        ta_ = slice(act_tiles[0], act_tiles[-1] + 1)
        op('dve', lambda e: e.tensor_tensor(out=rank[:, ta_, :], in0=rank[:, ta_, :], in1=bc(pstart.unsqueeze(1), [128, NTa, 32]), op=ALU.add), r=['rank', 'ev'], w=['rank'])
        for kk, Mk in enumerate((M1, M2)):
            op('dve', lambda e: e.tensor_tensor(out=Mk[:, ta_, :], in0=Mk[:, ta_, :], in1=rank[:, ta_, :], op=ALU.mult), r=['M', 'rank'], w=['M'])
            op('dve', lambda e: e.tensor_reduce(out=slotf[:, ta_, kk], in_=Mk[:, ta_, :], axis=AX.X, op=ALU.add), r=['M'], w=['slotf'])
        op('dve', lambda e: e.tensor_copy(out=slot_i[:, ta_, :], in_=slotf[:, ta_, :]), r=['slotf'], w=['slot_i'])
        op('dve', lambda e: e.tensor_tensor(out=cmpb[:, 0:NB, :], in0=bc(pend.unsqueeze(1), [128, NB, 32]), in1=bc(thr[:, 0:NB].unsqueeze(2), [128, NB, 32]), op=ALU.is_le), r=['ev', 'thr'], w=['cmpb', 'wm0', 'wm1'])
        op('dve', lambda e: e.tensor_reduce(out=bef[:, 0:NB], in_=cmpb[:, 0:NB, :], axis=AX.X, op=ALU.add), r=['cmpb'], w=['bef'])
        op('dve', lambda e: e.tensor_scalar(out=bef[:, 0:NB], in0=bef[:, 0:NB], scalar1=float(NE - 1), scalar2=None, op0=ALU.min), r=['bef'], w=['bef'])
        op('dve', lambda e: e.scalar_tensor_tensor(out=idxf1[:, 0:NB, :], in0=bc(bef[:, 0:NB].unsqueeze(2), [128, NB, 8]), scalar=1024.0, in1=bc(base1.unsqueeze(1), [128, NB, 8]), op0=ALU.mult, op1=ALU.add), r=['bef', 'base'], w=['idxf1'])
        op('dve', lambda e: e.tensor_scalar(out=idxf1[:, 0:NB, :], in0=idxf1[:, 0:NB, :], scalar1=float(l * NE * D), scalar2=None, op0=ALU.add), r=['idxf1'], w=['idxf1'])
        same = cmpb.rearrange("p a b -> p (a b)")[:, 0:NB - 2]
        op('dve', lambda e: e.tensor_tensor(out=same, in0=bef[:, 2:NB], in1=bef[:, 0:NB - 2], op=ALU.is_equal), r=['bef', 'cmpb'], w=['cmpb'])
        op('dve', lambda e: e.tensor_scalar(out=same, in0=same, scalar1=SKIPV, scalar2=None, op0=ALU.mult), r=['cmpb'], w=['cmpb'])
        op('dve', lambda e: e.tensor_tensor(out=idxf1[:, 2:NB, :], in0=idxf1[:, 2:NB, :], in1=bc(same.unsqueeze(2), [128, NB - 2, 8]), op=ALU.add), r=['cmpb', 'idxf1'], w=['idxf1'])
        op('dve', lambda e: e.tensor_copy(out=idx1[:, 0:NB, :], in_=idxf1[:, 0:NB, :]), r=['idxf1'], w=['idx1'])
        op('dve', lambda e: e.scalar_tensor_tensor(out=idxf2[:, 0:NB, :], in0=bc(bef[:, 0:NB].unsqueeze(2), [128, NB, 4]), scalar=512.0, in1=bc(base2.unsqueeze(1), [128, NB, 4]), op0=ALU.mult, op1=ALU.add), r=['bef', 'base'], w=['idxf2'])
        op('dve', lambda e: e.tensor_scalar(out=idxf2[:, 0:NB, :], in0=idxf2[:, 0:NB, :], scalar1=float(l * NE * 512), scalar2=None, op0=ALU.add), r=['idxf2'], w=['idxf2'])
        op('dve', lambda e: e.tensor_tensor(out=idxf2[:, 2:NB, :], in0=idxf2[:, 2:NB, :], in1=bc(same.unsqueeze(2), [128, NB - 2, 4]), op=ALU.add), r=['cmpb', 'idxf2'], w=['idxf2'])
        op('dve', lambda e: e.tensor_copy(out=idx2[:, 0:NB, :], in_=idxf2[:, 0:NB, :]), r=['idxf2'], w=['idx2'])
        for t in act_tiles:
            for kk in range(2):
                P.idma(xg_d, bass.IndirectOffsetOnAxis(ap=slot_i[:, t, kk:kk + 1], axis=0), ftok[:, t, :], None, reads=['ftok%d' % t, 'slot_i'], writes=['xg_%d_%d' % (t, kk)])
        xg_keys = ['xg_%d_%d' % (t, kk) for t in act_tiles for kk in range(2)]
        P.barrier()
        A.reset(b1_mark)
        w1b = [A.alloc([128, 8, 512], BF16) for _ in range(2)]
        w3b = [A.alloc([128, 8, 512], BF16) for _ in range(2)]
        w2b = [A.alloc([128, 4, D], BF16) for _ in range(2)]
        xb = [A.alloc([128, D], BF16) for _ in range(2)]
        fTb = [A.alloc([128, 8, 128], BF16) for _ in range(2)]
        s1b = [A.alloc([128, 512], BF16) for _ in range(2)]
        ga = [A.alloc([128, 4, 128], BF16) for _ in range(2)]
        ybuf = [A.alloc([128, D], F32) for _ in range(2)]
        print('SBUF B2 top', A.cur, A.top)
        w1rows = w1_d.rearrange("l e r n -> (l e r) n")
        if 'bnd' not in DBG:
            DBG['bnd'] = (nc.gpsimd.alloc_register('bnd1'), nc.gpsimd.alloc_register('bnd2'))
            nc.gpsimd.reg_mov(DBG['bnd'][0], NL * NE * D - 1)
            nc.gpsimd.reg_mov(DBG['bnd'][1], NL * NE * 512 - 1)
        bnd1, bnd2 = DBG['bnd']
        w3rows = w3_d.rearrange("l e r n -> (l e r) n")
        w2rows = w2_d.rearrange("l e r n -> (l e r) n")
        pending = None

        def make_y(b, b_):
            def f():
                for n in range(2):
                    ns = slice(n * 512, (n + 1) * 512)
                    py = PB[4 + n]
                    pyk = 'pb%d' % (4 + n)
                    for j in range(4):
                        mm(py, ga[b_][:, j, :], w2b[b_][:, j, ns], j == 0, j == 3, ['ga%d' % b_, 'w2_%d_%d' % (b_, j)], [pyk])
                    if n == 0:
                        op('dve', lambda e: e.tensor_copy(out=ybuf[b_][:, ns], in_=py), r=[pyk], w=['ybuf%d' % b_])
                    else:
                        op('act', lambda e: e.activation(out=ybuf[b_][:, ns], in_=py, func=AF.Copy), r=[pyk], w=['ybuf%d' % b_])
                P.dma('sp', yg_d[b * 128:(b + 1) * 128, :], ybuf[b_], reads=['ybuf%d' % b_], writes=['yg%d' % b])
            return f

        for b in range(NB):
            b_ = b % 2
            for k in range(8):
                P.idma(w1b[b_][:, k, :], None, w1rows, bass.IndirectOffsetOnAxis(ap=idx1[:, b, k:k + 1], axis=0), reads=['idx1'], writes=['w1_%d_%d' % (b_, k)], bounds=bnd1)
            for k in range(8):
                P.idma(w3b[b_][:, k, :], None, w3rows, bass.IndirectOffsetOnAxis(ap=idx1[:, b, k:k + 1], axis=0), reads=['idx1'], writes=['w3_%d_%d' % (b_, k)], bounds=bnd1)
            for j in range(4):
                P.idma(w2b[b_][:, j, :], None, w2rows, bass.IndirectOffsetOnAxis(ap=idx2[:, b, j:j + 1], axis=0), reads=['idx2'], writes=['w2_%d_%d' % (b_, j)], bounds=bnd2)
            P.dma('sp', xb[b_], xg_d[b * 128:(b + 1) * 128, :], reads=(xg_keys if b < 2 else []), writes=['xb%d' % b_])
            pst = PBH[6 + b_].rearrange("p (k n) -> p k n", k=8)
            for k in range(8):
                op('pe', lambda e: e.transpose(pst[:, k, :], xb[b_][:, k * 128:(k + 1) * 128], identb), r=['xb%d' % b_, 'identb'], w=['pb%d' % (6 + b_)])
            op('act', lambda e: e.activation(out=fTb[b_], in_=pst, func=AF.Copy), r=['pb%d' % (6 + b_)], w=['fTb%d' % b_])
            ph1 = PB[0 + 2 * b_].rearrange("p (j n) -> p j n", j=4)
            ph3 = PB[1 + 2 * b_].rearrange("p (j n) -> p j n", j=4)
            k1 = 'pb%d' % (0 + 2 * b_)
            k3 = 'pb%d' % (1 + 2 * b_)
            for j in range(4):
                for k in range(8):
                    mm(ph1[:, j, :], w1b[b_][:, k, j * 128:(j + 1) * 128], fTb[b_][:, k, :], k == 0, k == 7, ['w1_%d_%d' % (b_, k), 'fTb%d' % b_], [k1])
                for k in range(8):
                    mm(ph3[:, j, :], w3b[b_][:, k, j * 128:(j + 1) * 128], fTb[b_][:, k, :], k == 0, k == 7, ['w3_%d_%d' % (b_, k), 'fTb%d' % b_], [k3])
            if pending is not None:
                pending()
                pending = None
            op('act', lambda e: e.activation(out=s1b[b_], in_=PB[0 + 2 * b_], func=AF.Silu), r=[k1], w=['s1_%d' % b_])
            op('dve', lambda e: e.tensor_tensor(out=ga[b_].rearrange("p j n -> p (j n)"), in0=PB[1 + 2 * b_], in1=s1b[b_], op=ALU.mult), r=[k3, 's1_%d' % b_], w=['ga%d' % b_])
            pending = make_y(b, b_)
        if pending is not None:
            pending()
        P.barrier()
        A.reset(b1_mark)
        y1 = [A.alloc([128, D], F32) for _ in range(2)]
        y2 = [A.alloc([128, D], F32) for _ in range(2)]
        for ti, t in enumerate(act_tiles):
            r_ = 0 if t < 16 else 1
            b_ = ti % 2
            P.idma(y1[b_], None, yg_d, bass.IndirectOffsetOnAxis(ap=slot_i[:, t, 0:1], axis=0), reads=[], writes=['y1_%d' % b_])
            P.idma(y2[b_], None, yg_d, bass.IndirectOffsetOnAxis(ap=slot_i[:, t, 1:2], axis=0), reads=[], writes=['y2_%d' % b_])
            op('dve', lambda e: e.tensor_scalar(out=y1[b_], in0=y1[b_], scalar1=gate2[:, t, 0:1], scalar2=None, op0=ALU.mult), r=['y1_%d' % b_], w=['y1_%d' % b_])
            op('dve', lambda e: e.scalar_tensor_tensor(out=y2[b_], in0=y2[b_], scalar=gate2[:, t, 1:2], in1=y1[b_], op0=ALU.mult, op1=ALU.add), r=['y1_%d' % b_, 'y2_%d' % b_], w=['y2_%d' % b_])
            op('pool', lambda e: e.tensor_tensor(out=y2[b_], in0=y2[b_], in1=gb[:, 1, r_, :], op=ALU.mult), r=['y2_%d' % b_, 'gb'], w=['y2_%d' % b_])
            op('pool', lambda e: e.tensor_tensor(out=xs[:, t, :], in0=xs[:, t, :], in1=y2[b_], op=ALU.add), r=['y2_%d' % b_, 'xs%d' % t], w=['xs%d' % t])
        P.barrier()
        P.mute = False
        A.reset(lay_mark)

    xor = xo_d.rearrange("(t p) d -> p t d", p=128)
    for i in range(4):
        P.dma('sp', xor[:, 4 * i:4 * i + 4, :], xs[:, 4 * i:4 * i + 4, :], reads=['xs%d' % t for t in range(4 * i, 4 * i + 4)], writes=['xo'])
    P.dma('sp', co_d.rearrange("(t p) d -> p t d", p=128), xs[:, 16:18, :], reads=['xs16', 'xs17'], writes=['co'])
    P.finish('sp')
    return nc


def _consts():
    t = np.arange(T)
    rows = (t // 64).astype(np.float32)
    cols = (t % 64).astype(np.float32)
    out = {}
    for d in (64, 48):
        nf = d // 4
        freqs = (10000.0 ** (-np.arange(nf, dtype=np.float32) / nf)).astype(np.float32)
        ar = rows[:, None] * freqs[None, :]
        ac = cols[:, None] * freqs[None, :]
        cos = np.concatenate([np.cos(ar), np.cos(ar), np.cos(ac), np.cos(ac)], axis=1).astype(np.float32)
        sin = np.concatenate([-np.sin(ar), np.sin(ar), -np.sin(ac), np.sin(ac)], axis=1).astype(np.float32)
        cos = np.concatenate([cos, np.ones((C, d), np.float32)], axis=0)
        sin = np.concatenate([sin, np.zeros((C, d), np.float32)], axis=0)
        out['cos%d' % d] = np.ascontiguousarray(cos.reshape(NT, 128, d).transpose(1, 0, 2))
        out['sin%d' % d] = np.ascontiguousarray(sin.reshape(NT, 128, d).transpose(1, 0, 2))
    out['identf'] = np.eye(128, dtype=np.float32)
    s = np.arange(128)
    out['trif'] = (s[:, None] <= s[None, :]).astype(np.float32)
    out['trib'] = (s[:, None] >= s[None, :]).astype(np.float32)
    out['tris'] = (s[:, None] < s[None, :]).astype(np.float32)
    out['thr'] = np.ascontiguousarray(np.broadcast_to((128.0 * np.arange(104, dtype=np.float32))[None, :], (128, 104)))
    out['base1'] = (np.arange(8)[None, :] * 128 + np.arange(128)[:, None]).astype(np.float32)
    out['base2'] = (np.arange(4)[None, :] * 128 + np.arange(128)[:, None]).astype(np.float32)
    return out


_WNAMES = ['norm1_g', 'norm2_g', 'w_mod', 'b_mod', 'w_in', 'w_out', 'diff_q_norm', 'diff_k_norm', 'diff_lambda',
           'diff_subln', 'gqa_q_norm', 'gqa_k_norm', 'mlstm_conv_w', 'mlstm_conv_b', 'mlstm_gate_b', 'mlstm_head_norm',
           'moe_wg', 'moe_bg', 'moe_we', 'moe_be', 'moe_w1', 'moe_w3', 'moe_w2']
_PROG_CACHE = {}


def kernel(x, c, ctx, c_ctx, **w):
    x = np.asarray(x, np.float32)
    ctx = np.asarray(ctx, np.float32)
    c = np.asarray(c, np.float32)
    c_ctx = np.asarray(c_ctx, np.float32)
    w = {k: np.asarray(v, np.float32) for k, v in w.items()}
    consts = _consts()
    NL = N_LAYERS_PER_LAUNCH
    lam_all = np.array([[0.8 - 0.6 * math.exp(-0.3 * l), 1.0 - (0.8 - 0.6 * math.exp(-0.3 * l))] for l in range(DEPTH)], np.float32)
    xcur = [np.ascontiguousarray(x[b]) for b in range(8)]
    ccur = [np.ascontiguousarray(ctx[b]) for b in range(8)]
    ccs = [np.ascontiguousarray(np.stack([c[b], c_ctx], axis=0)) for b in range(8)]
    for l0 in range(0, DEPTH, NL):
        key = (NL, NL == 1)
        if key not in _PROG_CACHE:
            _PROG_CACHE[key] = build_program(NL, do_ctx_last=(NL == 1))
        nc = _PROG_CACHE[key]
        wl = {k: np.ascontiguousarray(w[k][l0:l0 + NL]) for k in _WNAMES}
        wl['lamc'] = np.ascontiguousarray(lam_all[l0:l0 + NL])
        in_maps = []
        for b in range(8):
            m = {'x': xcur[b], 'ctx': ccur[b], 'cc': ccs[b]}
            m.update(wl)
            m.update(consts)
            in_maps.append(m)
        res = run_bass_kernel_spmd(nc, in_maps, core_ids=list(range(8)))
        xcur = [np.asarray(res.results[b]['xo'], np.float32) for b in range(8)]
        ccur = [np.asarray(res.results[b]['co'], np.float32) for b in range(8)]
    return np.stack(xcur, axis=0).astype(np.float32)
```

```python
import math
import numpy as np
import concourse.bass as bass
import concourse.mybir as mybir
from concourse.bass_utils import run_bass_kernel_spmd

F32 = mybir.dt.float32
BF16 = mybir.dt.bfloat16
I32 = mybir.dt.int32
ALU = mybir.AluOpType
AF = mybir.ActivationFunctionType
AX = mybir.AxisListType

D = 1024
T = 2048
C = 256
NT = 18
DEPTH = 4
EPS = 1e-6
NE = 32
CHUNKS = [[0, 1, 2, 3], [4, 5, 6, 7], [8, 9, 10, 11], [12, 13, 14, 15], [16, 17]]
N_LAYERS_PER_LAUNCH = 4
PHASES = {'mod', 'A1', 'A2', 'B'}
DBG = {}


class Prog:
    def __init__(self, nc, ndma=56):
        self.nc = nc
        self.E = {'pe': nc.tensor, 'act': nc.scalar, 'dve': nc.vector, 'pool': nc.gpsimd, 'sp': nc.sync}
        self.sem = {k: nc.alloc_semaphore(name='s_' + k) for k in self.E}
        self.cnt = {k: 0 for k in self.E}
        self.dsem = [nc.alloc_semaphore(name='d%d' % i) for i in range(ndma)]
        self.dcnt = [0] * ndma
        self.dnext = 0
        self.dnext_pool = 0
        self.lastw = {}
        self.rd = {}
        self.waited = {k: {} for k in self.E}
        self.nwait = 0
        self.mute = False

    def _wait(self, eng, dep):
        s, v = dep
        key = id(s)
        if eng == 'pe' and s is self.sem['pe']:
            return
        if self.waited[eng].get(key, 0) >= v:
            return
        self.E[eng].wait_ge(s, v)
        self.nwait += 1
        self.waited[eng][key] = v

    def _deps(self, eng, reads, writes):
        for k in reads:
            if k in self.lastw:
                self._wait(eng, self.lastw[k])
        for k in writes:
            if k in self.lastw:
                self._wait(eng, self.lastw[k])
            for d in self.rd.get(k, ()):
                self._wait(eng, d)

    def _mark(self, tok, reads, writes):
        for k in writes:
            self.lastw[k] = tok
            self.rd[k] = []
        for k in reads:
            if k not in writes:
                self.rd.setdefault(k, []).append(tok)

    @staticmethod
    def _excl(reads, writes):
        pr = [k for k in reads if k.startswith('pb')]
        if pr:
            reads = [k for k in reads if not k.startswith('pb')]
            writes = list(writes) + [k for k in pr if k not in writes]
        return reads, writes

    def op(self, eng, fn, reads=(), writes=()):
        if self.mute:
            return
        reads, writes = self._excl(reads, writes)
        self._deps(eng, reads, writes)
        inst = fn(self.E[eng])
        self.cnt[eng] += 1
        inst.then_inc(self.sem[eng], 1)
        self._mark((self.sem[eng], self.cnt[eng]), reads, writes)

    def dma(self, eng, out, in_, reads=(), writes=(), **kw):
        if self.mute:
            return
        half = 12
        if eng == 'pool':
            i = half + self.dnext_pool
            self.dnext_pool = (self.dnext_pool + 1) % (len(self.dsem) - half)
        else:
            i = self.dnext
            self.dnext = (self.dnext + 1) % half
        if self.dcnt[i] > 0:
            self._wait(eng, (self.dsem[i], self.dcnt[i]))
        self._deps(eng, reads, writes)
        inst = self.E[eng].dma_start(out=out, in_=in_, **kw)
        self.dcnt[i] += 16
        inst.then_inc(self.dsem[i], 16)
        self._mark((self.dsem[i], self.dcnt[i]), reads, writes)

    def idma(self, out, out_off, in_, in_off, reads=(), writes=()):
        if self.mute:
            return
        eng = 'pool'
        half = 12
        i = half + self.dnext_pool
        self.dnext_pool = (self.dnext_pool + 1) % (len(self.dsem) - half)
        if self.dcnt[i] > 0:
            self._wait(eng, (self.dsem[i], self.dcnt[i]))
        self._deps(eng, reads, writes)
        inst = self.E[eng].indirect_dma_start(out=out, out_offset=out_off, in_=in_, in_offset=in_off)
        self.dcnt[i] += 16
        inst.then_inc(self.dsem[i], 16)
        self._mark((self.dsem[i], self.dcnt[i]), reads, writes)

    def barrier(self):
        for e in self.E:
            for f in self.E:
                if f != e and self.cnt[f] > 0:
                    self._wait(e, (self.sem[f], self.cnt[f]))
            for i, s in enumerate(self.dsem):
                if self.dcnt[i] > 0:
                    self._wait(e, (s, self.dcnt[i]))
        self.lastw = {}
        self.rd = {}

    def finish(self, eng='sp'):
        for f in self.E:
            if f != eng and self.cnt[f] > 0:
                self._wait(eng, (self.sem[f], self.cnt[f]))
        for i, s in enumerate(self.dsem):
            if self.dcnt[i] > 0:
                self._wait(eng, (s, self.dcnt[i]))


class Arena:
    def __init__(self, nc):
        self.nc = nc
        self.cur = (nc.sbuf_base + 63) // 64 * 64
        self.top = nc.sbuf_top
        self.n = 0

    def alloc(self, shape, dtype):
        per = 1
        for s in shape[1:]:
            per *= s
        per *= 2 if dtype == BF16 else 4
        per = (per + 63) // 64 * 64
        off = self.cur
        self.cur += per
        assert self.cur <= self.top, "SBUF overflow: need %d more bytes" % (self.cur - self.top)
        self.n += 1
        return self.nc.alloc_sbuf_tensor_at("sb%d" % self.n, list(shape), dtype, offset=off).ap()

    def mark(self):
        return self.cur

    def reset(self, m):
        self.cur = m


def bc(ap, shape):
    return ap.to_broadcast(list(shape))


def build_program(NL, do_ctx_last):
    nc = bass.Bass("TRN2", target_bir_lowering=False)

    def din(name, shape):
        return nc.dram_tensor(name, list(shape), F32, kind="ExternalInput").ap()

    x_d = din("x", [T, D])
    ctx_d = din("ctx", [C, D])
    cc_d = din("cc", [2, D])
    n1_d = din("norm1_g", [NL, D])
    n2_d = din("norm2_g", [NL, D])
    wmod_d = din("w_mod", [NL, D, 6 * D])
    bmod_d = din("b_mod", [NL, 6 * D])
    win_d = din("w_in", [NL, D, 2832])
    wout_d = din("w_out", [NL, D, D])
    dqn_d = din("diff_q_norm", [NL, 48])
    dkn_d = din("diff_k_norm", [NL, 48])
    dlam_d = din("diff_lambda", [NL, 4, 48])
    dsub_d = din("diff_subln", [NL, 96])
    gqn_d = din("gqa_q_norm", [NL, 64])
    gkn_d = din("gqa_k_norm", [NL, 64])
    cw_d = din("mlstm_conv_w", [NL, 3, 512])
    cb_d = din("mlstm_conv_b", [NL, 512])
    gtb_d = din("mlstm_gate_b", [NL, 16])
    hng_d = din("mlstm_head_norm", [NL, 256])
    wg_d = din("moe_wg", [NL, D, 4])
    bg_d = din("moe_bg", [NL, 4])
    we_d = din("moe_we", [NL, D, 32])
    be_d = din("moe_be", [NL, 32])
    w1_d = din("moe_w1", [NL, NE, D, 512])
    w3_d = din("moe_w3", [NL, NE, D, 512])
    w2_d = din("moe_w2", [NL, NE, 512, D])
    lamc_d = din("lamc", [NL, 2])
    identf_d = din("identf", [128, 128])
    trif_d = din("trif", [128, 128])
    trib_d = din("trib", [128, 128])
    cos64_d = din("cos64", [128, NT, 64])
    sin64_d = din("sin64", [128, NT, 64])
    cos48_d = din("cos48", [128, NT, 48])
    sin48_d = din("sin48", [128, NT, 48])
    xo_d = nc.dram_tensor("xo", [T, D], F32, kind="ExternalOutput").ap()
    co_d = nc.dram_tensor("co", [C, D], F32, kind="ExternalOutput").ap()
    hmd_d = nc.dram_tensor("hmd", [NT * 128, 256], BF16, kind="Internal").ap()
    NBMAX = NT * 2 + NE
    xg_d = nc.dram_tensor("xg", [NBMAX * 128, D], BF16, kind="Internal").ap()
    yg_d = nc.dram_tensor("yg", [NBMAX * 128, D], F32, kind="Internal").ap()
    tris_d = din("tris", [128, 128])
    thr_d = din("thr", [128, 104])
    base1_d = din("base1", [128, 8])
    base2_d = din("base2", [128, 4])

    P = Prog(nc)
    A = Arena(nc)
    PB = [nc.alloc_psum_tensor("pb%d" % i, [128, 512], F32).ap() for i in range(8)]
    PBH = [p.bitcast(BF16) for p in PB]

    def op(eng, fn, r=(), w=()):
        P.op(eng, fn, r, w)

    def mm(out, lhsT, rhs, start, stop, r, w, skip=False):
        P.op('pe', lambda e: e.matmul(out, lhsT=lhsT, rhs=rhs, start=start, stop=stop, skip_group_check=skip), r, w)

    xs = A.alloc([128, NT, D], F32)
    identb = A.alloc([128, 128], BF16)
    identf = A.alloc([128, 128], F32)
    trif = A.alloc([128, 128], F32)
    trib = A.alloc([128, 128], F32)
    ones64 = A.alloc([128, 128], F32)
    trisb = A.alloc([128, 128], BF16)
    onesb = A.alloc([128, 128], BF16)
    thr = A.alloc([128, 104], F32)
    base1 = A.alloc([128, 8], F32)
    base2 = A.alloc([128, 4], F32)
    cos64 = A.alloc([128, NT, 64], BF16)
    sin64 = A.alloc([128, NT, 64], BF16)
    cos48 = A.alloc([128, NT, 48], BF16)
    sin48 = A.alloc([128, NT, 48], BF16)
    scol = A.alloc([128, 2, 8], BF16)
    gb = A.alloc([128, 2, 2, D], BF16)
    modA = A.alloc([128, 2, 8, 2], F32)
    modB = A.alloc([128, 2, 8, 2], F32)
    sv = A.alloc([128, 900], F32)
    cwc = A.alloc([128, 3, 4], F32)
    cbc = A.alloc([128, 4], F32)
    kc = A.alloc([128, 8], F32)
    tiny = A.alloc([128, 128], F32)
    SV_DQN, SV_DKN, SV_DSUB, SV_GQN, SV_GKN, SV_HNG, SV_GTB, SV_RB, SV_LAMC, SV_DLAM = 0, 48, 96, 192, 256, 320, 576, 592, 628, 640
    pers_mark = A.mark()
    DBG.update(xs=xs, modA=modA, modB=modB, gb=gb, sv=sv, scol=scol, hmd=hmd_d)

    ld_mark = A.mark()
    stg = A.alloc([128, NT, 64], F32)
    for (dst, src, d) in ((cos64, cos64_d, 64), (sin64, sin64_d, 64), (cos48, cos48_d, 48), (sin48, sin48_d, 48)):
        P.dma('pool', dst, src, writes=['tbl'])
    P.dma('sp', identf, identf_d, writes=['identf'])
    P.dma('pool', identb, identf_d, writes=['identb'])
    P.dma('sp', trif, trif_d, writes=['trif'])
    P.dma('sp', trib, trib_d, writes=['trib'])
    P.dma('pool', trisb, tris_d, writes=['trisb'])
    P.dma('sp', thr, thr_d, writes=['thr'])
    P.dma('sp', base1, base1_d, writes=['base'])
    P.dma('sp', base2, base2_d, writes=['base'])
    op('pool', lambda e: e.memset(onesb, 1.0), w=['onesb'])
    op('pool', lambda e: e.memset(ones64, 1.0), w=['ones64'])
    op('pool', lambda e: e.memset(kc[:, 0:1], EPS), w=['kc'])
    op('pool', lambda e: e.memset(kc[:, 1:2], math.log(0.125)), w=['kc'])
    op('pool', lambda e: e.memset(kc[:, 2:3], 1.0), w=['kc'])
    op('pool', lambda e: e.memset(kc[:, 3:4], 0.0), w=['kc'])
    xr = x_d.rearrange("(t p) d -> p t d", p=128)
    for i in range(4):
        P.dma('sp', xs[:, 4 * i:4 * i + 4, :], xr[:, 4 * i:4 * i + 4, :], writes=['xs%d' % t for t in range(4 * i, 4 * i + 4)])
    P.dma('sp', xs[:, 16:18, :], ctx_d.rearrange("(t p) d -> p t d", p=128), writes=['xs16', 'xs17'])
    ccf = A.alloc([128, 2, 8], F32)
    for r_i in range(2):
        P.dma('sp', ccf[:, r_i, :], cc_d[r_i].rearrange("(k p) -> p k", p=128), writes=['ccf'], allow_slow_non_contiguous=True)
    op('act', lambda e: e.activation(out=scol, in_=ccf, func=AF.Silu), r=['ccf'], w=['scol'])
    zt = A.alloc([128, 4, D], BF16)
    op('pool', lambda e: e.memset(zt, 0.0), w=['zt'])
    xgz = xg_d.rearrange("(g b p) d -> g p b d", p=128, b=4)
    for gi_ in range(NBMAX // 4):
        P.dma('sp', xgz[gi_], zt, reads=['zt'], writes=['xgz%d' % gi_])
    A.reset(ld_mark)
    P.barrier()

    eps_ap = kc[:, 0:1]
    ln8_ap = kc[:, 1:2]

    def rstd_from_ss(ss_ap, n, dim, rk, wk):
        op('act', lambda e: e.activation(out=ss_ap, in_=ss_ap, func=AF.Ln, scale=1.0 / dim, bias=eps_ap), r=rk, w=wk)
        op('act', lambda e: e.activation(out=ss_ap, in_=ss_ap, func=AF.Exp, scale=-0.5), r=wk, w=wk)

    def norm_T(dst, tiles, which, scr):
        junk, xn, ss = scr[:3]
        jk = scr[3] if len(scr) > 3 else 'nT_junk'
        for i, t in enumerate(tiles):
            r = 0 if t < 16 else 1
            op('pool', lambda e: e.memset(ss, 0.0), w=['nT_ss'])
            op('act', lambda e: e.activation(out=junk, in_=xs[:, t, :], func=AF.Square, accum_out=ss), r=['xs%d' % t, 'nT_ss'], w=[jk, 'nT_ss'])
            rstd_from_ss(ss, 1, D, ['nT_ss'], ['nT_ss'])
            op('dve', lambda e: e.tensor_scalar(out=xn, in0=xs[:, t, :], scalar1=ss, scalar2=None, op0=ALU.mult), r=['xs%d' % t, 'nT_ss'], w=['nT_xn'])
            pst = PBH[7].rearrange("p (k n) -> p k n", k=8)
            for k in range(8):
                op('pe', lambda e: e.transpose(pst[:, k, :], xn[:, k * 128:(k + 1) * 128], identb), r=['nT_xn', 'identb'], w=['pb7'])
            for k in range(8):
                eng = 'dve' if t % 2 == 0 else 'act'
                o = dst[:, k, i * 128:(i + 1) * 128]
                if eng == 'dve':
                    op('dve', lambda e: e.tensor_scalar(out=o, in0=pst[:, k, :], scalar1=modA[:, which, k, r:r + 1], scalar2=modB[:, which, k, r:r + 1], op0=ALU.mult, op1=ALU.add), r=['pb7', 'mod'], w=['hT'])
                else:
                    op('act', lambda e: e.activation(out=o, in_=pst[:, k, :], func=AF.Identity, scale=modA[:, which, k, r:r + 1], bias=modB[:, which, k, r:r + 1]), r=['pb7', 'mod'], w=['hT'])

    def qknr(ps_ap, U, d, gain, cos_t, sin_t, out_ap, scr, psk, split=None):
        s1, s2, s3, ss = scr
        q4 = d // 4
        s1v = s1[:, 0:U * d].rearrange("p (u d) -> p u d", u=U)
        s2v = s2[:, 0:U * d].rearrange("p (u d) -> p u d", u=U)
        s3v = s3[:, 0:U * d].rearrange("p (u d) -> p u d", u=U)
        ssv = ss[:, 0:U]
        psv = ps_ap.rearrange("p (u d) -> p u d", u=U)
        op('act', lambda e: e.activation(out=s1[:, 0:U * d], in_=ps_ap, func=AF.Square), r=[psk], w=['q_s1'])
        op('dve', lambda e: e.tensor_reduce(out=ssv, in_=s1v, axis=AX.X, op=ALU.add), r=['q_s1'], w=['q_ss'])
        rstd_from_ss(ssv, U, d, ['q_ss'], ['q_ss'])
        op('dve', lambda e: e.tensor_tensor(out=s1v, in0=psv, in1=bc(ssv.unsqueeze(2), [128, U, d]), op=ALU.mult), r=[psk, 'q_ss', 'q_s1'], w=['q_s1'])
        op('pool', lambda e: e.tensor_tensor(out=s1v, in0=s1v, in1=bc(gain.unsqueeze(1), [128, U, d]), op=ALU.mult), r=['q_s1', 'sv'], w=['q_s1'])
        op('pool', lambda e: e.tensor_tensor(out=s2v, in0=s1v, in1=bc(cos_t.unsqueeze(1), [128, U, d]), op=ALU.mult), r=['q_s1', 'tbl'], w=['q_s2'])
        x5 = s1[:, 0:U * d].rearrange("p (u a b c) -> p u a b c", u=U, a=2, b=2)
        t5 = s3[:, 0:U * d].rearrange("p (u a b c) -> p u a b c", u=U, a=2, b=2)
        sn5 = sin_t.rearrange("p (a b c) -> p a b c", a=2, b=2)
        for b_ in range(2):
            op('pool', lambda e: e.tensor_tensor(out=t5[:, :, :, b_, :], in0=x5[:, :, :, 1 - b_, :], in1=bc(sn5[:, :, b_, :].unsqueeze(1), [128, U, 2, q4]), op=ALU.mult), r=['q_s1', 'tbl'], w=['q_s3'])
        if split is not None:
            s2o = s2v.rearrange("p (k j) d -> p k j d", k=split)
            s3o = s3v.rearrange("p (k j) d -> p k j d", k=split)
        else:
            s2o, s3o = s2v, s3v
        op('dve', lambda e: e.tensor_tensor(out=out_ap, in0=s2o, in1=s3o, op=ALU.add), r=['q_s2', 'q_s3'], w=['q_out'])

    for l in range(NL):
        last = (l == NL - 1)
        upd_ctx = (not last) or do_ctx_last
        lay_mark = A.mark()
        for (off, src, n) in ((SV_DQN, dqn_d[l], 48), (SV_DKN, dkn_d[l], 48), (SV_DSUB, dsub_d[l], 96), (SV_GQN, gqn_d[l], 64),
                              (SV_GKN, gkn_d[l], 64), (SV_HNG, hng_d[l], 256), (SV_GTB, gtb_d[l], 16), (SV_RB, bg_d[l], 4),
                              (SV_RB + 4, be_d[l], 32), (SV_LAMC, lamc_d[l], 2)):
            P.dma('sp', sv[:, off:off + n], src.partition_broadcast(128), writes=['sv'])
        P.dma('sp', sv[:, SV_DLAM:SV_DLAM + 192], dlam_d[l].rearrange("a b -> (a b)").partition_broadcast(128), writes=['sv'])
        for j_ in range(3):
            P.dma('sp', cwc[:, j_, :], cw_d[l, j_].rearrange("(ct p) -> p ct", p=128), writes=['cwc'], allow_slow_non_contiguous=True)
        P.dma('sp', cbc, cb_d[l].rearrange("(ct p) -> p ct", p=128), writes=['cwc'], allow_slow_non_contiguous=True)
        dl = sv[:, SV_DLAM:SV_DLAM + 192].rearrange("p (a b) -> p a b", a=4)
        op('dve', lambda e: e.tensor_tensor(out=tiny[:, 0:96].rearrange("p (a b) -> p a b", a=2), in0=dl[:, 0:4:2, :], in1=dl[:, 1:4:2, :], op=ALU.mult), r=['sv'], w=['tiny'])
        op('dve', lambda e: e.tensor_reduce(out=tiny[:, 96:98], in_=tiny[:, 0:96].rearrange("p (a b) -> p a b", a=2), axis=AX.X, op=ALU.add), r=['tiny'], w=['tiny'])
        op('act', lambda e: e.activation(out=tiny[:, 96:98], in_=tiny[:, 96:98], func=AF.Exp), r=['tiny'], w=['tiny'])
        op('dve', lambda e: e.tensor_tensor(out=tiny[:, 98:99], in0=tiny[:, 96:97], in1=tiny[:, 97:98], op=ALU.subtract), r=['tiny'], w=['tiny'])
        op('dve', lambda e: e.tensor_scalar(out=sv[:, 840:841], in0=tiny[:, 98:99], scalar1=sv[:, SV_LAMC:SV_LAMC + 1], scalar2=-1.0, op0=ALU.add, op1=ALU.mult), r=['tiny', 'sv'], w=['sv'])
        op('dve', lambda e: e.tensor_scalar(out=sv[:, SV_DSUB:SV_DSUB + 96], in0=sv[:, SV_DSUB:SV_DSUB + 96], scalar1=sv[:, SV_LAMC + 1:SV_LAMC + 2], scalar2=None, op0=ALU.mult), r=['sv'], w=['sv'])
        neglam = sv[:, 840:841]

        P.mute = 'mod' not in PHASES
        m_mark = A.mark()
        wm = [A.alloc([128, 8, 512], BF16) for _ in range(2)]
        srep = A.alloc([128, 2, 8, 128], BF16)
        op('dve', lambda e: e.tensor_copy(out=srep, in_=bc(scol.unsqueeze(3), [128, 2, 8, 128])), r=['scol'], w=['srep'])
        bmb = A.alloc([128, 512], F32)
        ncol = A.alloc([128, 2, 8], F32)
        bcol = A.alloc([128, 48], F32)
        rowst = A.alloc([64, 128], F32)
        P.dma('sp', rowst[0:48, :], bmod_d[l].rearrange("(t p) -> t p", p=128), writes=['rowst'])
        P.dma('sp', rowst[48:56, :], n1_d[l].rearrange("(t p) -> t p", p=128), writes=['rowst'])
        P.dma('sp', rowst[56:64, :], n2_d[l].rearrange("(t p) -> t p", p=128), writes=['rowst'])
        mm(PB[6][:, 0:64], rowst, identf[0:64, 0:64], True, True, ['rowst', 'identf'], ['pb6'])
        op('dve', lambda e: e.tensor_copy(out=bcol, in_=PB[6][:, 0:48]), r=['pb6'], w=['bcol'])
        op('dve', lambda e: e.tensor_copy(out=ncol.rearrange("p a k -> p (a k)"), in_=PB[6][:, 48:64]), r=['pb6'], w=['ncol'])
        wmr = wmod_d[l].rearrange("(k p) n -> p k n", p=128)
        pcol = PB[5][:, 0:64].rearrange("p (c r) -> p c r", r=2)
        colidx = {0: 0, 1: 4, 2: 8, 3: 12, 6: 16, 7: 20, 8: 24, 9: 28}
        for j in range(12):
            wb_ = wm[j % 2]
            P.dma('pool', wb_, wmr[:, :, j * 512:(j + 1) * 512], writes=['wm%d' % (j % 2)])
            if j in colidx:
                for ct in range(4):
                    ci = colidx[j] + ct
                    for k in range(8):
                        mm(pcol[:, ci, :], wb_[:, k, ct * 128:(ct + 1) * 128], scol[:, :, k], k == 0, k == 7, ['wm%d' % (j % 2), 'scol'], ['pb5'])
            else:
                which = 0 if j < 6 else 1
                half = j % 2 if j < 6 else (j - 10)
                P.dma('sp', bmb, bmod_d[l, j * 512:(j + 1) * 512].partition_broadcast(128), writes=['bmb'])
                for r in range(2):
                    for k in range(8):
                        mm(PB[6], srep[:, r, k, :], wb_[:, k, :], k == 0, k == 7, ['wm%d' % (j % 2), 'srep'], ['pb6'])
                    op('dve', lambda e: e.tensor_tensor(out=gb[:, which, r, half * 512:(half + 1) * 512], in0=PB[6], in1=bmb, op=ALU.add), r=['pb6', 'bmb'], w=['gb'])
        for which, (sh_c, sc_c, sh_b, sc_b) in enumerate(((0, 8, 0, 8), (16, 24, 24, 32))):
            op('dve', lambda e: e.tensor_tensor(out=modB[:, which], in0=pcol[:, sh_c:sh_c + 8, :], in1=bc(bcol[:, sh_b:sh_b + 8].unsqueeze(2), [128, 8, 2]), op=ALU.add), r=['pb5', 'bcol'], w=['mod'])
            op('dve', lambda e: e.tensor_tensor(out=modA[:, which], in0=pcol[:, sc_c:sc_c + 8, :], in1=bc(bcol[:, sc_b:sc_b + 8].unsqueeze(2), [128, 8, 2]), op=ALU.add), r=['pb5', 'bcol', 'mod'], w=['mod'])
            op('dve', lambda e: e.tensor_scalar(out=modA[:, which], in0=modA[:, which], scalar1=1.0, scalar2=None, op0=ALU.add), r=['mod'], w=['mod'])
            op('dve', lambda e: e.tensor_tensor(out=modA[:, which], in0=modA[:, which], in1=bc(ncol[:, which, :].unsqueeze(2), [128, 8, 2]), op=ALU.mult), r=['mod', 'ncol'], w=['mod'])
        P.barrier()
        A.reset(m_mark)

        P.mute = 'A1' not in PHASES and 'A1a' not in PHASES
        a_mark = A.mark()
        RAWW = 2304 + 8
        raw = A.alloc([128, 4, RAWW], BF16)
        qkT = A.alloc([128, 4, NT * 128], BF16)
        ktok = A.alloc([128, NT, 256], BF16)
        Vm = A.alloc([128, NT, 4, 66], BF16)
        osig = A.alloc([128, NT, 256], BF16)
        gts = A.alloc([128, NT, 16], F32)
        lfm = A.alloc([128, NT, 8], F32)
        hbm = A.alloc([128, NT, 256], BF16)
        a1_mark = A.mark()
        DBG.update(raw=raw, qkT=qkT, ktok=ktok, Vm=Vm, osig=osig, gts=gts, lfm=lfm, hbm=hbm)
        hT = A.alloc([128, 8, 512], BF16)
        nscr = (A.alloc([128, D], BF16), A.alloc([128, D], BF16), A.alloc([128, 1], F32))
        wA = A.alloc([128, 8, 1040], BF16)
        sgs = A.alloc([128, 256], F32)
        print("SBUF A1 proj top", A.cur, A.top)

        winr = win_d[l].rearrange("(k p) n -> p k n", p=128)
        P.dma('pool', wA[:, :, 0:512], winr[:, :, 1792:2304], writes=['wA'])
        P.dma('pool', wA[:, :, 512:1040], winr[:, :, 2304:2832], writes=['wA'])
        op('pool', lambda e: e.memset(raw, 0.0), w=['raw'])
        op('pool', lambda e: e.memset(Vm, 1.0), w=['Vm'])

        def tokoff(t):
            return (2 + t * 128) if t < 16 else (2052 + (t - 16) * 128)

        for ci, tiles in enumerate(CHUNKS):
            ntok = len(tiles) * 128
            import os as _os
            _sk = set(_os.environ.get('DBGSKIP', '').split(','))
            _m0 = P.mute
            P.mute = _m0 or 'nT' in _sk
            norm_T(hT, tiles, 0, nscr)
            P.mute = _m0 or 'fm' in _sk
            for ct in range(4):
                pb_ = PB[ct % 2]
                for k in range(8):
                    mm(pb_[:, 0:ntok], wA[:, k, ct * 128:(ct + 1) * 128], hT[:, k, 0:ntok], k == 0, k == 7, ['wA', 'hT'], ['pb%d' % (ct % 2)])
                o0 = tokoff(tiles[0])
                op('act', lambda e: e.activation(out=raw[:, ct, o0:o0 + ntok], in_=pb_[:, 0:ntok], func=AF.Copy), r=['pb%d' % (ct % 2)], w=['raw'])
            P.mute = _m0 or 'tm' in _sk
            for i, t in enumerate(tiles):
                pb_ = PB[2 + i % 2]
                pk = 'pb%d' % (2 + i % 2)
                for k in range(8):
                    mm(pb_, hT[:, k, i * 128:(i + 1) * 128], wA[:, k, 512:1024], k == 0, k == 7, ['wA', 'hT'], [pk])
                P.mute = _m0 or 'tm' in _sk or 'vcopy' in _sk
                op('dve', lambda e: e.tensor_copy(out=Vm[:, t, :, 0:64], in_=pb_[:, 0:256].rearrange("p (h d) -> p h d", h=4)), r=[pk], w=['Vm'])
                P.mute = _m0 or 'tm' in _sk or 'sig' in _sk
                op('act', lambda e: e.activation(out=sgs, in_=pb_[:, 256:512], func=AF.Exp, scale=-1.0), r=[pk], w=['sgs'])
                op('dve', lambda e: e.tensor_scalar(out=sgs, in0=sgs, scalar1=1.0, scalar2=None, op0=ALU.add), r=['sgs'], w=['sgs'])
                op('dve', lambda e: e.reciprocal(out=sgs, in_=sgs), r=['sgs'], w=['sgs'])
                op('pool', lambda e: e.tensor_copy(out=osig[:, t, :], in_=sgs), r=['sgs'], w=['osig'])
                P.mute = _m0 or 'tm' in _sk or 'gates' in _sk
                for k in range(8):
                    mm(PB[4][:, 0:16], hT[:, k, i * 128:(i + 1) * 128], wA[:, k, 1024:1040], k == 0, k == 7, ['wA', 'hT'], ['pb4'])
                op('dve', lambda e: e.tensor_tensor(out=gts[:, t, :], in0=PB[4][:, 0:16], in1=sv[:, SV_GTB:SV_GTB + 16], op=ALU.add), r=['pb4', 'sv'], w=['gts'])
            P.mute = _m0
        P.barrier()
        A.reset(a1_mark)
        Cst = A.alloc([128, 2, 2, 66], F32)
        Cb = A.alloc([128, 2, 2, 66], BF16)
        WT = [A.alloc([128, 4, 128], BF16) for _ in range(2)]
        Vp = A.alloc([128, 4, 66], BF16)
        cscr = [A.alloc([128, 512], F32) for _ in range(2)]
        msm = A.alloc([128, 64], F32)
        Ctmp = A.alloc([128, 2, 66], F32)
        hsc = [A.alloc([128, 256], F32) for _ in range(2)]
        P.mute = 'A1' not in PHASES and 'A1b' not in PHASES
        op('pool', lambda e: e.memset(Cst, 0.0), w=['Cst'])
        op('pool', lambda e: e.memset(Cb, 0.0), w=['Cb'])
        g4 = gts.rearrange("p t (a h) -> p t a h", a=4)
        l4 = lfm.rearrange("p t (a h) -> p t a h", a=2)
        op('act', lambda e: e.activation(out=l4, in_=g4[:, :, 1:4:2, :], func=AF.Exp, scale=-1.0), r=['gts'], w=['lfm'])
        op('act', lambda e: e.activation(out=l4, in_=l4, func=AF.Ln, bias=kc[:, 2:3]), r=['lfm'], w=['lfm'])
        op('dve', lambda e: e.tensor_scalar(out=l4, in0=l4, scalar1=-1.0, scalar2=None, op0=ALU.mult), r=['lfm'], w=['lfm'])
        for ct in range(4):
            for (o0, n) in ((2, 512), (514, 512), (1026, 512), (1538, 512), (2052, 256)):
                s0, s1 = cscr
                dst0 = o0 - 2 if o0 < 2052 else 2048
                op('pool', lambda e: e.tensor_scalar(out=s0[:, 0:n], in0=raw[:, ct, o0 - 1:o0 - 1 + n], scalar1=cwc[:, 0, ct:ct + 1], scalar2=None, op0=ALU.mult), r=['raw', 'cwc'], w=['cs0'])
                op('dve', lambda e: e.scalar_tensor_tensor(out=s1[:, 0:n], in0=raw[:, ct, o0:o0 + n], scalar=cwc[:, 1, ct:ct + 1], in1=s0[:, 0:n], op0=ALU.mult, op1=ALU.add), r=['raw', 'cwc', 'cs0'], w=['cs1'])
                op('dve', lambda e: e.scalar_tensor_tensor(out=s0[:, 0:n], in0=raw[:, ct, o0 + 1:o0 + 1 + n], scalar=cwc[:, 2, ct:ct + 1], in1=s1[:, 0:n], op0=ALU.mult, op1=ALU.add), r=['raw', 'cwc', 'cs1', 'cs0'], w=['cs0'])
                op('act', lambda e: e.activation(out=qkT[:, ct, dst0:dst0 + n], in_=s0[:, 0:n], func=AF.Silu, bias=cbc[:, ct:ct + 1]), r=['cs0', 'cwc'], w=['qkT'])
        for t in range(NT):
            for pr in range(2):
                pst = PBH[pr][:, 0:128]
                op('pe', lambda e: e.transpose(pst, qkT[:, 2 + pr, t * 128:(t + 1) * 128], identb), r=['qkT', 'identb'], w=['pb%d' % pr])
                op('dve' if pr == 0 else 'act', (lambda e: e.tensor_copy(out=ktok[:, t, pr * 128:(pr + 1) * 128], in_=pst)) if pr == 0 else
                   (lambda e: e.activation(out=ktok[:, t, pr * 128:(pr + 1) * 128], in_=pst, func=AF.Copy)), r=['pb%d' % pr], w=['ktok'])
        P.mute = 'A1' not in PHASES and 'A1c' not in PHASES
        for di in (1, 0):
            tri = trif if di == 0 else trib
            trik = 'trif' if di == 0 else 'trib'
            order = [16, 17] + list(range(16)) if di == 0 else [17, 16] + list(range(15, -1, -1))
            for t in order:
                tcols = slice(t * 128, (t + 1) * 128)
                lf = lfm[:, t, di * 4:di * 4 + 4]
                li = gts[:, t, di * 8:di * 8 + 4]
                mm(PB[0][:, 0:4], tri, lf, True, True, [trik, 'lfm'], ['pb0'])
                mm(PB[0][:, 8:12], ones64, lf, True, True, ['ones64', 'lfm'], ['pb0'])
                cs_ = msm[:, 0:4]
                eb_ = msm[:, 4:8]
                eB_ = msm[:, 8:12]
                op('dve', lambda e: e.tensor_tensor(out=cs_, in0=li, in1=PB[0][:, 0:4], op=ALU.subtract), r=['gts', 'pb0'], w=['m_cs'])
                op('act', lambda e: e.activation(out=cs_, in_=cs_, func=AF.Exp, bias=ln8_ap), r=['m_cs'], w=['m_cs'])
                op('act', lambda e: e.activation(out=eb_, in_=PB[0][:, 0:4], func=AF.Exp), r=['pb0'], w=['m_eb'])
                op('act', lambda e: e.activation(out=eB_, in_=PB[0][:, 8:12], func=AF.Exp), r=['pb0'], w=['m_eB'])
                wt = WT[t % 2]
                wk = 'WT%d' % (t % 2)
                ps_e = PB[1].rearrange("p (h n) -> p h n", h=4)
                ps_o = PB[4].rearrange("p (h n) -> p h n", h=4)
                for h in range(4):
                    pb0 = (h % 2) * 64
                    pss = ps_e if h % 2 == 0 else ps_o
                    mm(pss[:, h // 2, :], qkT[pb0:pb0 + 64, 2 + h // 2, tcols], qkT[pb0:pb0 + 64, h // 2, tcols], True, True, ['qkT'], ['pb1' if h % 2 == 0 else 'pb4'])
                for h in range(4):
                    pss = ps_e if h % 2 == 0 else ps_o
                    op('dve', lambda e: e.scalar_tensor_tensor(out=wt[:, h, :], in0=pss[:, h // 2, :], scalar=cs_[:, h:h + 1], in1=tri, op0=ALU.mult, op1=ALU.mult), r=['pb1' if h % 2 == 0 else 'pb4', 'm_cs', trik], w=[wk])
                op('pool', lambda e: e.tensor_tensor(out=Vp, in0=Vm[:, t], in1=bc(cs_.unsqueeze(2), [128, 4, 66]), op=ALU.mult), r=['Vm', 'm_cs'], w=['Vp'])
                pso = PB[2][:, 0:260].rearrange("p (h n) -> p h n", h=4)
                for h in range(4):
                    pb0 = (h % 2) * 64
                    mm(pso[:, h, :], wt[:, h, :], Vm[:, t, h, 0:65], True, False, [wk, 'Vm'], ['pb2'])
                    mm(pso[:, h, :], qkT[pb0:pb0 + 64, h // 2, tcols], Cb[pb0:pb0 + 64, di, h // 2, 0:65], False, True, ['qkT', 'Cb'], ['pb2'])
                psu = PB[3][:, 0:260].rearrange("p (h n) -> p h n", h=4)
                for h in range(4):
                    mm(psu[:, h, :], ktok[:, t, (h // 2) * 128:(h // 2 + 1) * 128], Vp[:, h, 0:65], True, True, ['ktok', 'Vp'], ['pb3'])
                for hh in range(2):
                    ps_ = slice(hh * 64, hh * 64 + 64)
                    op('dve', lambda e: e.tensor_tensor(out=Ctmp[ps_, :, 0:65], in0=psu[ps_, hh:4:2, :], in1=Cst[ps_, di, :, 0:65], op=ALU.add), r=['pb3', 'Cst'], w=['Ctmp'])
                    op('dve', lambda e: e.tensor_tensor(out=Cst[ps_, di, :, 0:65], in0=Ctmp[ps_, :, 0:65], in1=bc(eB_[ps_, hh:4:2].unsqueeze(2), [64, 2, 65]), op=ALU.mult), r=['Ctmp', 'm_eB'], w=['Cst'])
                op('pool', lambda e: e.tensor_copy(out=Cb[:, di], in_=Cst[:, di]), r=['Cst'], w=['Cb'])
                den = msm[:, 12:16]
                nd = msm[:, 16:20]
                op('dve', lambda e: e.tensor_tensor(out=den, in0=pso[:, :, 64], in1=eb_, op=ALU.mult), r=['pb2', 'm_eb'], w=['m_den'])
                op('dve', lambda e: e.tensor_scalar(out=nd, in0=den, scalar1=-1.0, scalar2=None, op0=ALU.mult), r=['m_den'], w=['m_nd'])
                op('dve', lambda e: e.tensor_tensor(out=den, in0=den, in1=nd, op=ALU.max), r=['m_den', 'm_nd'], w=['m_den'])
                op('dve', lambda e: e.tensor_scalar(out=den, in0=den, scalar1=1.0, scalar2=None, op0=ALU.max), r=['m_den'], w=['m_den'])
                op('dve', lambda e: e.reciprocal(out=den, in_=den), r=['m_den'], w=['m_den'])
                op('dve', lambda e: e.tensor_tensor(out=den, in0=den, in1=eb_, op=ALU.mult), r=['m_den', 'm_eb'], w=['m_den'])
                if di == 1:
                    op('dve', lambda e: e.tensor_tensor(out=hbm[:, t, :].rearrange("p (h d) -> p h d", h=4), in0=pso[:, :, 0:64], in1=bc(den.unsqueeze(2), [128, 4, 64]), op=ALU.mult), r=['pb2', 'm_den'], w=['hbm%d' % t])
                else:
                    h0, h1 = hsc
                    h0v = h0.rearrange("p (h d) -> p h d", h=4)
                    h1v = h1.rearrange("p (h d) -> p h d", h=4)
                    ssn = msm[:, 20:24]
                    op('dve', lambda e: e.tensor_tensor(out=h0v, in0=pso[:, :, 0:64], in1=bc(den.unsqueeze(2), [128, 4, 64]), op=ALU.mult), r=['pb2', 'm_den'], w=['h0'])
                    op('pool', lambda e: e.tensor_tensor(out=h0, in0=h0, in1=hbm[:, t, :], op=ALU.add), r=['h0', 'hbm%d' % t], w=['h0'])
                    op('pool', lambda e: e.tensor_tensor(out=h1, in0=h0, in1=h0, op=ALU.mult), r=['h0'], w=['h1'])
                    op('dve', lambda e: e.tensor_reduce(out=ssn, in_=h1v, axis=AX.X, op=ALU.add), r=['h1'], w=['m_ssn'])
                    rstd_from_ss(ssn, 4, 64, ['m_ssn'], ['m_ssn'])
                    op('dve', lambda e: e.tensor_tensor(out=h1v, in0=h0v, in1=bc(ssn.unsqueeze(2), [128, 4, 64]), op=ALU.mult), r=['h0', 'm_ssn', 'h1'], w=['h1'])
                    op('pool', lambda e: e.tensor_tensor(out=h1, in0=h1, in1=sv[:, SV_HNG:SV_HNG + 256], op=ALU.mult), r=['h1', 'sv'], w=['h1'])
                    op('pool', lambda e: e.tensor_tensor(out=hbm[:, t, :], in0=h1, in1=osig[:, t, :], op=ALU.mult), r=['h1', 'osig', 'hbm%d' % t], w=['hbm%d' % t])
                    P.dma('sp', hmd_d[t * 128:(t + 1) * 128, :], hbm[:, t, :], reads=['hbm%d' % t], writes=['hmd%d' % t])
        P.barrier()
        A.reset(a_mark)

        P.mute = 'A2' not in PHASES
        hT = A.alloc([128, 8, 512], BF16)
        wq = A.alloc([128, 8, 768], BF16)
        wkv = A.alloc([128, 8, 1024], BF16)
        dkT = A.alloc([128, 4, NT * 128], BF16)
        gkT = A.alloc([128, NT * 128], BF16)
        Vd = A.alloc([128, NT, 4, 98], BF16)
        Vg = A.alloc([128, NT, 2, 66], BF16)
        dqT = A.alloc([128, 4, 512], BF16)
        gqT = A.alloc([128, 3, 512], BF16)
        Eb = [A.alloc([128, 512], BF16) for _ in range(4)]
        pad = [A.alloc([128, 8, 64], BF16) for _ in range(2)]
        mix = A.alloc([128, 4, D], BF16)
        mixT = A.alloc([128, 8, 128], BF16)
        qs = (A.alloc([128, 384], F32), A.alloc([128, 384], F32), A.alloc([128, 384], F32), A.alloc([128, 8], F32))
        osA = A.alloc([128, 4, 96], F32)
        osB = A.alloc([128, 4, 96], F32)
        ysc = A.alloc([128, 512], F32)
        nscr = (ysc.bitcast(BF16), A.alloc([128, D], BF16), A.alloc([128, 1], F32), 'ysc')
        ar = A.alloc([128, 8], F32)
        DBG.update(dkT=dkT, gkT=gkT, Vd=Vd, Vg=Vg, dqT=dqT, gqT=gqT, mix=mix)
        print('SBUF A2 top', A.cur, A.top)

        P.dma('pool', wq[:, :, 0:384], winr[:, :, 0:384], writes=['wq'])
        P.dma('pool', wq[:, :, 384:768], winr[:, :, 1152:1536], writes=['wq'])
        P.dma('pool', wkv[:, :, 0:768], winr[:, :, 384:1152], writes=['wkv'])
        P.dma('pool', wkv[:, :, 768:1024], winr[:, :, 1536:1792], writes=['wkv'])
        op('pool', lambda e: e.memset(Vd, 1.0), w=['Vd'])
        op('pool', lambda e: e.memset(Vg, 1.0), w=['Vg'])
        for p_ in pad:
            op('pool', lambda e: e.memset(p_, 0.0), w=['pad0', 'pad1'])

        def to_T(pad_ap, ncolt, dst_fn, psb, psk, pk):
            for j in range(ncolt):
                op('pe', lambda e: e.transpose(PBH[psb][:, j * 128:(j + 1) * 128], pad_ap[:, j * 128:(j + 1) * 128], identb), r=[pk, 'identb'], w=[psk])
            for j in range(ncolt):
                if psb % 2 == 0:
                    op('dve', lambda e: e.tensor_copy(out=dst_fn(j), in_=PBH[psb][:, j * 128:(j + 1) * 128]), r=[psk], w=['T_dst'])
                else:
                    op('act', lambda e: e.activation(out=dst_fn(j), in_=PBH[psb][:, j * 128:(j + 1) * 128], func=AF.Copy), r=[psk], w=['T_dst'])

        for ci, tiles in enumerate(CHUNKS):
            norm_T(hT, tiles, 0, nscr)
            for i, t in enumerate(tiles):
                hs = slice(i * 128, (i + 1) * 128)
                for k in range(8):
                    mm(PB[0][:, 0:384], hT[:, k, hs], wkv[:, k, 0:384], k == 0, k == 7, ['wkv', 'hT'], ['pb0'])
                qknr(PB[0][:, 0:384], 8, 48, sv[:, SV_DKN:SV_DKN + 48], cos48[:, t, :], sin48[:, t, :], pad[0][:, :, 0:48], qs, 'pb0')
                to_T(pad[0].rearrange("p u d -> p (u d)"), 4, lambda j: dkT[:, j, t * 128:(t + 1) * 128], 4, 'pb4', 'q_out')
                for k in range(8):
                    mm(PB[1][:, 0:384], hT[:, k, hs], wkv[:, k, 384:768], k == 0, k == 7, ['wkv', 'hT'], ['pb1'])
                op('act', lambda e: e.activation(out=Vd[:, t, :, 0:96], in_=PB[1][:, 0:384].rearrange("p (h d) -> p h d", h=4), func=AF.Copy), r=['pb1'], w=['Vd'])
                for k in range(8):
                    mm(PB[2][:, 0:256], hT[:, k, hs], wkv[:, k, 768:1024], k == 0, k == 7, ['wkv', 'hT'], ['pb2'])
                qknr(PB[2][:, 0:128], 2, 64, sv[:, SV_GKN:SV_GKN + 64], cos64[:, t, :], sin64[:, t, :], pad[1][:, 0:2, :], qs, 'pb2')
                to_T(pad[1].rearrange("p u d -> p (u d)"), 1, lambda j: gkT[:, t * 128:(t + 1) * 128], 5, 'pb5', 'q_out')
                op('act', lambda e: e.activation(out=Vg[:, t, :, 0:64], in_=PB[2][:, 128:256].rearrange("p (h d) -> p h d", h=2), func=AF.Copy), r=['pb2'], w=['Vg'])
        woutr = wout_d[l].rearrange("(k p) n -> p k n", p=128)
        P.dma('pool', wkv, woutr, reads=['wkv'], writes=['wkv'])

        sc48 = 48.0 ** -0.5
        sc64 = 0.125
        qchunks = CHUNKS if upd_ctx else CHUNKS[:4]
        ei = 0
        for ci, tiles in enumerate(qchunks):
            nq = len(tiles)
            nqt = nq * 128
            r_ = 0 if tiles[0] < 16 else 1
            ktiles = list(range(NT)) if r_ == 0 else [16, 17]
            norm_T(hT, tiles, 0, nscr)
            for i, t in enumerate(tiles):
                hs = slice(i * 128, (i + 1) * 128)
                for k in range(8):
                    mm(PB[0][:, 0:384], hT[:, k, hs], wq[:, k, 0:384], k == 0, k == 7, ['wq', 'hT'], ['pb0'])
                qknr(PB[0][:, 0:384], 8, 48, sv[:, SV_DQN:SV_DQN + 48], cos48[:, t, :], sin48[:, t, :], pad[0][:, :, 0:48], qs, 'pb0')
                to_T(pad[0].rearrange("p u d -> p (u d)"), 4, lambda j: dqT[:, j, hs], 4, 'pb4', 'q_out')
                for k in range(8):
                    mm(PB[1][:, 0:384], hT[:, k, hs], wq[:, k, 384:768], k == 0, k == 7, ['wq', 'hT'], ['pb1'])
                qknr(PB[1][:, 0:384], 6, 64, sv[:, SV_GQN:SV_GQN + 64], cos64[:, t, :], sin64[:, t, :], pad[1][:, 0:6, :].rearrange("p (j k) d -> p k j d", k=2), qs, 'pb1', split=2)
                to_T(pad[1].rearrange("p u d -> p (u d)"), 3, lambda j: gqT[:, j, hs], 5, 'pb5', 'q_out')
                P.dma('sp', mix[:, i, 768:1024], hmd_d[t * 128:(t + 1) * 128, :], reads=['hmd%d' % t], writes=['mix'])
            items = []
            nkt = len(ktiles)

            def diff_post(h, b0, b1):
                def f():
                    po0 = PB[b0][:, 0:nq * 97].rearrange("p (q n) -> p q n", q=nq)
                    po1 = PB[b1][:, 0:nq * 97].rearrange("p (q n) -> p q n", q=nq)
                    k0, k1 = 'pb%d' % b0, 'pb%d' % b1
                    r1 = ar[:, 0:nq]
                    r2 = ar[:, 4:4 + nq]
                    oa = osA[:, 0:nq, :]
                    ob = osB[:, 0:nq, :]
                    op('dve', lambda e: e.reciprocal(out=r1, in_=po0[:, :, 96]), r=[k0], w=['a_r1'])
                    op('dve', lambda e: e.reciprocal(out=r2, in_=po1[:, :, 96]), r=[k1], w=['a_r2'])
                    op('dve', lambda e: e.tensor_scalar(out=r2, in0=r2, scalar1=neglam, scalar2=None, op0=ALU.mult), r=['a_r2', 'sv'], w=['a_r2'])
                    op('dve', lambda e: e.tensor_tensor(out=oa, in0=po0[:, :, 0:96], in1=bc(r1.unsqueeze(2), [128, nq, 96]), op=ALU.mult), r=[k0, 'a_r1'], w=['osA'])
                    op('dve', lambda e: e.tensor_tensor(out=ob, in0=po1[:, :, 0:96], in1=bc(r2.unsqueeze(2), [128, nq, 96]), op=ALU.mult), r=[k1, 'a_r2'], w=['osB'])
                    op('pool', lambda e: e.tensor_tensor(out=oa, in0=oa, in1=ob, op=ALU.add), r=['osA', 'osB'], w=['osA'])
                    op('pool', lambda e: e.tensor_tensor(out=ob, in0=oa, in1=oa, op=ALU.mult), r=['osA', 'osB'], w=['osB'])
                    op('dve', lambda e: e.tensor_reduce(out=r1, in_=ob, axis=AX.X, op=ALU.add), r=['osB', 'a_r1'], w=['a_r1'])
                    rstd_from_ss(r1, nq, 96, ['a_r1'], ['a_r1'])
                    op('dve', lambda e: e.tensor_tensor(out=ob, in0=oa, in1=bc(r1.unsqueeze(2), [128, nq, 96]), op=ALU.mult), r=['osA', 'a_r1', 'osB'], w=['osB'])
                    op('pool', lambda e: e.tensor_tensor(out=mix[:, 0:nq, h * 96:(h + 1) * 96], in0=ob, in1=bc(sv[:, SV_DSUB:SV_DSUB + 96].unsqueeze(1), [128, nq, 96]), op=ALU.mult), r=['osB', 'sv'], w=['mix'])
                return f

            def gqa_post(g, bk):
                def f():
                    pso = PB[bk][:, 0:nq * 65].rearrange("p (q n) -> p q n", q=nq)
                    pok = 'pb%d' % bk
                    rg = ar[:, 0:nq] if g % 2 == 0 else ar[:, 4:4 + nq]
                    rgk = 'a_r1' if g % 2 == 0 else 'a_r2'
                    op('dve', lambda e: e.reciprocal(out=rg, in_=pso[:, :, 64]), r=[pok], w=[rgk])
                    op('dve', lambda e: e.tensor_tensor(out=mix[:, 0:nq, 384 + g * 64:384 + (g + 1) * 64], in0=pso[:, :, 0:64], in1=bc(rg.unsqueeze(2), [128, nq, 64]), op=ALU.mult), r=[pok, rgk], w=['mix'])
                return f

            for h in range(4):
                banks = (2, 3) if h % 2 == 0 else (4, 5)
                for half in range(2):
                    u = 2 * h + half
                    for kidx, kt in enumerate(ktiles):
                        post = diff_post(h, banks[0], banks[1]) if (half == 1 and kidx == nkt - 1) else None
                        items.append(dict(kind='d', lhs=dkT[(u % 2) * 64:(u % 2) * 64 + 48, u // 2, kt * 128:(kt + 1) * 128],
                                          rhs=dqT[(u % 2) * 64:(u % 2) * 64 + 48, u // 2, 0:nqt], sc=sc48,
                                          V=Vd[:, kt, h, 0:97], vk='Vd', W=97, bank=banks[half], first=(kidx == 0), last=(kidx == nkt - 1), post=post))
            for g in range(6):
                kvh = g // 3
                bk = 2 + g % 4
                for kidx, kt in enumerate(ktiles):
                    post = gqa_post(g, bk) if kidx == nkt - 1 else None
                    items.append(dict(kind='g', lhs=gkT[kvh * 64:(kvh + 1) * 64, kt * 128:(kt + 1) * 128],
                                      rhs=gqT[kvh * 64:(kvh + 1) * 64, g % 3, 0:nqt], sc=sc64,
                                      V=Vg[:, kt, kvh, 0:65], vk='Vg', W=65, bank=bk, first=(kidx == 0), last=(kidx == nkt - 1), post=post))

            def emit_S(i):
                it = items[i]
                sb_ = (0, 1, 6)[i % 3]
                mm(PB[sb_][:, 0:nqt], it['lhs'], it['rhs'], True, True, ['T_dst'], ['pb%d' % sb_])

            def emit_EPV(i):
                it = items[i]
                sb_ = (0, 1, 6)[i % 3]
                eb_i = i % 4
                op('act', lambda e: e.activation(out=Eb[eb_i][:, 0:nqt], in_=PB[sb_][:, 0:nqt], func=AF.Exp, scale=it['sc']), r=['pb%d' % sb_], w=['E%d' % eb_i])
                pso = PB[it['bank']][:, 0:nq * it['W']].rearrange("p (q n) -> p q n", q=nq)
                for q in range(nq):
                    mm(pso[:, q, :], Eb[eb_i][:, q * 128:(q + 1) * 128], it['V'], it['first'] and q == 0, it['last'], ['E%d' % eb_i, it['vk']], ['pb%d' % it['bank']], skip=True)
                if it['post'] is not None:
                    it['post']()

            emit_S(0)
            emit_S(1)
            for i in range(len(items)):
                if i + 2 < len(items):
                    emit_S(i + 2)
                emit_EPV(i)
            for i, t in enumerate(tiles):
                pst = PBH[6].rearrange("p (k n) -> p k n", k=8)
                for k in range(8):
                    op('pe', lambda e: e.transpose(pst[:, k, :], mix[:, i, k * 128:(k + 1) * 128], identb), r=['mix', 'identb'], w=['pb6'])
                op('act', lambda e: e.activation(out=mixT, in_=pst, func=AF.Copy), r=['pb6'], w=['mixT'])
                for n in range(2):
                    ns = slice(n * 512, (n + 1) * 512)
                    for k in range(8):
                        mm(PB[7], mixT[:, k, :], wkv[:, k, ns], k == 0, k == 7, ['mixT', 'wkv'], ['pb7'])
                    op('dve', lambda e: e.tensor_tensor(out=ysc, in0=PB[7], in1=gb[:, 0, r_, ns], op=ALU.mult), r=['pb7', 'gb'], w=['ysc'])
                    op('pool', lambda e: e.tensor_tensor(out=xs[:, t, ns], in0=xs[:, t, ns], in1=ysc, op=ALU.add), r=['ysc', 'xs%d' % t], w=['xs%d' % t])
        P.barrier()
        A.reset(a_mark)

        P.mute = 'B' not in PHASES
        act_tiles = list(range(NT)) if upd_ctx else list(range(16))
        NTa = len(act_tiles)
        NB = NTa * 2 + NE
        ftok = A.alloc([128, NT, D], BF16)
        slot_i = A.alloc([128, NT, 2], I32)
        gate2 = A.alloc([128, NT, 2], F32)
        blkexp_i = A.alloc([128, 104], I32)
        idx1 = A.alloc([128, 104, 8], I32)
        idx2 = A.alloc([128, 104, 4], I32)
        b1_mark = A.mark()
        modrow = A.alloc([128, 2, 2, D], F32)
        wm_mark = A.mark()
        wm2 = [A.alloc([128, 8, 512], BF16) for _ in range(2)]
        bmb2 = A.alloc([128, 512], F32)
        n2row = A.alloc([128, D], F32)
        srep = A.alloc([128, 2, 8, 128], BF16)
        fsc = A.alloc([128, D], F32)
        junkb = A.alloc([128, D], BF16)
        fTt = A.alloc([128, 8, 128], BF16)
        wr = A.alloc([128, 8, 36], BF16)
        rsc = A.alloc([128, 160], F32)
        ssb = A.alloc([128, 1], F32)
        Mb = A.alloc([128, NT, 32], BF16)
        M1 = A.alloc([128, NT, 32], F32)
        M2 = A.alloc([128, NT, 32], F32)
        rank = A.alloc([128, NT, 32], F32)
        Mcum = A.alloc([128, 32], BF16)
        ev = A.alloc([128, 6, 32], F32)
        slotf = A.alloc([128, NT, 2], F32)
        _sv = A.mark()
        A.reset(wm_mark)
        cmpb = A.alloc([128, 104, 32], F32)
        A.reset(_sv)
        bef = A.alloc([128, 104], F32)
        idxf1 = A.alloc([128, 104, 8], F32)
        idxf2 = A.alloc([128, 104, 4], F32)
        print('SBUF B1 top', A.cur, A.top)
        op('dve', lambda e: e.tensor_copy(out=srep, in_=bc(scol.unsqueeze(3), [128, 2, 8, 128])), r=['scol'], w=['srep'])
        P.dma('sp', n2row, n2_d[l].partition_broadcast(128), writes=['n2row'])
        P.dma('pool', wr[:, :, 0:4], wg_d[l].rearrange("(k p) n -> p k n", p=128), writes=['wr'])
        P.dma('pool', wr[:, :, 4:36], we_d[l].rearrange("(k p) n -> p k n", p=128), writes=['wr'])
        for j in (6, 7, 8, 9):
            wb_ = wm2[j % 2]
            ab = 1 if j < 8 else 0
            half = j % 2
            P.dma('pool', wb_, wmr[:, :, j * 512:(j + 1) * 512], writes=['wm%d' % (j % 2)])
            P.dma('sp', bmb2, bmod_d[l, j * 512:(j + 1) * 512].partition_broadcast(128), writes=['bmb'])
            for r in range(2):
                for k in range(8):
                    mm(PB[6], srep[:, r, k, :], wb_[:, k, :], k == 0, k == 7, ['wm%d' % (j % 2), 'srep'], ['pb6'])
                op('dve', lambda e: e.tensor_tensor(out=modrow[:, ab, r, half * 512:(half + 1) * 512], in0=PB[6], in1=bmb2, op=ALU.add), r=['pb6', 'bmb'], w=['modrow'])
        for r in range(2):
            op('dve', lambda e: e.scalar_tensor_tensor(out=modrow[:, 0, r, :], in0=modrow[:, 0, r, :], scalar=1.0, in1=n2row, op0=ALU.add, op1=ALU.mult), r=['modrow', 'n2row'], w=['modrow'])
        for t in act_tiles:
            r_ = 0 if t < 16 else 1
            op('pool', lambda e: e.memset(ssb, 0.0), w=['ssb'])
            op('act', lambda e: e.activation(out=junkb, in_=xs[:, t, :], func=AF.Square, accum_out=ssb), r=['xs%d' % t, 'ssb'], w=['junkb', 'ssb'])
            rstd_from_ss(ssb, 1, D, ['ssb'], ['ssb'])
            op('dve', lambda e: e.scalar_tensor_tensor(out=fsc, in0=xs[:, t, :], scalar=ssb, in1=modrow[:, 0, r_, :], op0=ALU.mult, op1=ALU.mult), r=['xs%d' % t, 'ssb', 'modrow'], w=['fsc'])
            op('pool', lambda e: e.tensor_tensor(out=ftok[:, t, :], in0=fsc, in1=modrow[:, 1, r_, :], op=ALU.add), r=['fsc', 'modrow'], w=['ftok%d' % t])
            pst = PBH[7].rearrange("p (k n) -> p k n", k=8)
            for k in range(8):
                op('pe', lambda e: e.transpose(pst[:, k, :], ftok[:, t, k * 128:(k + 1) * 128], identb), r=['ftok%d' % t, 'identb'], w=['pb7'])
            op('act', lambda e: e.activation(out=fTt, in_=pst, func=AF.Copy), r=['pb7'], w=['fTt'])
            for k in range(8):
                mm(PB[0][:, 0:36], fTt[:, k, :], wr[:, k, :], k == 0, k == 7, ['fTt', 'wr'], ['pb0'])
            lg = rsc[:, 0:36]
            gmax = rsc[:, 36:37]
            ngmax = rsc[:, 37:38]
            gsum = rsc[:, 38:39]
            eg = rsc[:, 40:44]
            maskg = rsc[:, 44:48]
            em = rsc[:, 48:80]
            m1 = rsc[:, 80:81]
            em2 = rsc[:, 128:160]
            m2 = rsc[:, 81:82]
            dd = rsc[:, 82:83]
            p1 = gate2[:, t, 0:1]
            p2 = gate2[:, t, 1:2]
            mask1 = M1[:, t, :]
            mask2 = M2[:, t, :]
            rk = ['rsc']
            op('dve', lambda e: e.tensor_tensor(out=lg, in0=PB[0][:, 0:36], in1=sv[:, SV_RB:SV_RB + 36], op=ALU.add), r=['pb0', 'sv', 'rsc'], w=rk)
            op('dve', lambda e: e.tensor_reduce(out=gmax, in_=lg[:, 0:4], axis=AX.X, op=ALU.max), r=rk, w=rk)
            op('dve', lambda e: e.tensor_scalar(out=ngmax, in0=gmax, scalar1=-1.0, scalar2=None, op0=ALU.mult), r=rk, w=rk)
            op('pool', lambda e: e.memset(gsum, 0.0), r=rk, w=rk)
            op('act', lambda e: e.activation(out=eg, in_=lg[:, 0:4], func=AF.Exp, bias=ngmax, accum_out=gsum), r=rk, w=rk)
            op('dve', lambda e: e.reciprocal(out=gsum, in_=gsum), r=rk, w=rk)
            op('dve', lambda e: e.tensor_scalar(out=maskg, in0=lg[:, 0:4], scalar1=gmax, scalar2=None, op0=ALU.is_equal), r=rk, w=rk)
            op('dve', lambda e: e.tensor_scalar(out=maskg, in0=maskg, scalar1=-1.0, scalar2=1e30, op0=ALU.add, op1=ALU.mult), r=rk, w=rk)
            op('dve', lambda e: e.tensor_tensor(out=em.rearrange("p (g x) -> p g x", g=4), in0=lg[:, 4:36].rearrange("p (g x) -> p g x", g=4), in1=bc(maskg.unsqueeze(2), [128, 4, 8]), op=ALU.add), r=rk, w=rk)
            op('dve', lambda e: e.tensor_reduce(out=m1, in_=em, axis=AX.X, op=ALU.max), r=rk, w=rk)
            op('dve', lambda e: e.tensor_scalar(out=mask1, in0=em, scalar1=m1, scalar2=None, op0=ALU.is_equal), r=rk, w=rk + ['M'])
            op('dve', lambda e: e.scalar_tensor_tensor(out=em2, in0=mask1, scalar=-1e30, in1=em, op0=ALU.mult, op1=ALU.add), r=rk + ['M'], w=rk)
            op('dve', lambda e: e.tensor_reduce(out=m2, in_=em2, axis=AX.X, op=ALU.max), r=rk, w=rk)
            op('dve', lambda e: e.tensor_scalar(out=mask2, in0=em2, scalar1=m2, scalar2=None, op0=ALU.is_equal), r=rk, w=rk + ['M'])
            op('pool', lambda e: e.tensor_tensor(out=Mb[:, t, :], in0=mask1, in1=mask2, op=ALU.add), r=['M'], w=['Mb'])
            op('dve', lambda e: e.tensor_tensor(out=dd, in0=m2, in1=m1, op=ALU.subtract), r=rk, w=rk)
            op('act', lambda e: e.activation(out=dd, in_=dd, func=AF.Exp), r=rk, w=rk)
            op('dve', lambda e: e.tensor_scalar(out=p1, in0=dd, scalar1=1.0, scalar2=None, op0=ALU.add), r=rk, w=rk + ['gate2'])
            op('dve', lambda e: e.reciprocal(out=p1, in_=p1), r=['gate2'], w=['gate2'])
            op('dve', lambda e: e.tensor_tensor(out=p1, in0=p1, in1=gsum, op=ALU.mult), r=rk + ['gate2'], w=['gate2'])
            op('dve', lambda e: e.tensor_tensor(out=p2, in0=p1, in1=dd, op=ALU.mult), r=rk + ['gate2'], w=['gate2'])
        op('pool', lambda e: e.memset(Mcum, 0.0), w=['Mcum'])
        for ti, t in enumerate(act_tiles):
            pbk = 1 + ti % 2
            mm(PB[pbk][:, 0:32], trisb, Mb[:, t, :], True, False, ['trisb', 'Mb'], ['pb%d' % pbk])
            mm(PB[pbk][:, 0:32], onesb, Mcum, False, True, ['onesb', 'Mcum'], ['pb%d' % pbk])
            op('dve', lambda e: e.tensor_copy(out=rank[:, t, :], in_=PB[pbk][:, 0:32]), r=['pb%d' % pbk], w=['rank'])
            op('pool', lambda e: e.tensor_tensor(out=Mcum, in0=Mcum, in1=Mb[:, t, :], op=ALU.add), r=['Mcum', 'Mb'], w=['Mcum'])
        mm(PB[3][:, 0:32], onesb, Mcum, True, True, ['onesb', 'Mcum'], ['pb3'])
        cnt, nblk, scA, scB, pend, pstart = [ev[:, i, :] for i in range(6)]
        op('dve', lambda e: e.tensor_copy(out=cnt, in_=PB[3][:, 0:32]), r=['pb3'], w=['ev'])
        op('dve', lambda e: e.memset(nblk, 0.0), r=['ev'], w=['ev'])
        for j in range(NT):
            op('dve', lambda e: e.scalar_tensor_tensor(out=nblk, in0=cnt, scalar=128.0 * j, in1=nblk, op0=ALU.is_gt, op1=ALU.add), r=['ev'], w=['ev'])
        op('dve', lambda e: e.tensor_copy(out=scA, in_=nblk), r=['ev'], w=['ev'])
        src_, dst_ = scA, scB
        for sft in (1, 2, 4, 8, 16):
            op('dve', lambda e: e.tensor_copy(out=dst_, in_=src_), r=['ev'], w=['ev'])
            op('dve', lambda e: e.tensor_tensor(out=dst_[:, sft:32], in0=src_[:, sft:32], in1=src_[:, 0:32 - sft], op=ALU.add), r=['ev'], w=['ev'])
            src_, dst_ = dst_, src_
        op('dve', lambda e: e.tensor_scalar(out=pend, in0=src_, scalar1=128.0, scalar2=None, op0=ALU.mult), r=['ev'], w=['ev'])
        op('dve', lambda e: e.scalar_tensor_tensor(out=pstart, in0=nblk, scalar=-128.0, in1=pend, op0=ALU.mult, op1=ALU.add), r=['ev'], w=['ev'])
        ta_ = slice(act_tiles[0], act_tiles[-1] + 1)
        op('dve', lambda e: e.tensor_tensor(out=rank[:, ta_, :], in0=rank[:, ta_, :], in1=bc(pstart.unsqueeze(1), [128, NTa, 32]), op=ALU.add), r=['rank', 'ev'], w=['rank'])
        for kk, Mk in enumerate((M1, M2)):
            op('dve', lambda e: e.tensor_tensor(out=Mk[:, ta_, :], in0=Mk[:, ta_, :], in1=rank[:, ta_, :], op=ALU.mult), r=['M', 'rank'], w=['M'])
            op('dve', lambda e: e.tensor_reduce(out=slotf[:, ta_, kk], in_=Mk[:, ta_, :], axis=AX.X, op=ALU.add), r=['M'], w=['slotf'])
        op('dve', lambda e: e.tensor_copy(out=slot_i[:, ta_, :], in_=slotf[:, ta_, :]), r=['slotf'], w=['slot_i'])
        op('dve', lambda e: e.tensor_tensor(out=cmpb[:, 0:NB, :], in0=bc(pend.unsqueeze(1), [128, NB, 32]), in1=bc(thr[:, 0:NB].unsqueeze(2), [128, NB, 32]), op=ALU.is_le), r=['ev', 'thr'], w=['cmpb', 'wm0', 'wm1'])
        op('dve', lambda e: e.tensor_reduce(out=bef[:, 0:NB], in_=cmpb[:, 0:NB, :], axis=AX.X, op=ALU.add), r=['cmpb'], w=['bef'])
        op('dve', lambda e: e.tensor_scalar(out=bef[:, 0:NB], in0=bef[:, 0:NB], scalar1=float(NE - 1), scalar2=None, op0=ALU.min), r=['bef'], w=['bef'])
        op('dve', lambda e: e.scalar_tensor_tensor(out=idxf1[:, 0:NB, :], in0=bc(bef[:, 0:NB].unsqueeze(2), [128, NB, 8]), scalar=1024.0, in1=bc(base1.unsqueeze(1), [128, NB, 8]), op0=ALU.mult, op1=ALU.add), r=['bef', 'base'], w=['idxf1'])
        op('dve', lambda e: e.tensor_scalar(out=idxf1[:, 0:NB, :], in0=idxf1[:, 0:NB, :], scalar1=float(l * NE * D), scalar2=None, op0=ALU.add), r=['idxf1'], w=['idxf1'])
        op('dve', lambda e: e.tensor_copy(out=idx1[:, 0:NB, :], in_=idxf1[:, 0:NB, :]), r=['idxf1'], w=['idx1'])
        op('dve', lambda e: e.scalar_tensor_tensor(out=idxf2[:, 0:NB, :], in0=bc(bef[:, 0:NB].unsqueeze(2), [128, NB, 4]), scalar=512.0, in1=bc(base2.unsqueeze(1), [128, NB, 4]), op0=ALU.mult, op1=ALU.add), r=['bef', 'base'], w=['idxf2'])
        op('dve', lambda e: e.tensor_scalar(out=idxf2[:, 0:NB, :], in0=idxf2[:, 0:NB, :], scalar1=float(l * NE * 512), scalar2=None, op0=ALU.add), r=['idxf2'], w=['idxf2'])
        op('dve', lambda e: e.tensor_copy(out=idx2[:, 0:NB, :], in_=idxf2[:, 0:NB, :]), r=['idxf2'], w=['idx2'])
        for t in act_tiles:
            for kk in range(2):
                P.idma(xg_d, bass.IndirectOffsetOnAxis(ap=slot_i[:, t, kk:kk + 1], axis=0), ftok[:, t, :], None, reads=['ftok%d' % t, 'slot_i'], writes=['xg_%d_%d' % (t, kk)])
        xg_keys = ['xg_%d_%d' % (t, kk) for t in act_tiles for kk in range(2)]
        P.barrier()
        A.reset(b1_mark)
        w1b = [A.alloc([128, 8, 512], BF16) for _ in range(2)]
        w3b = [A.alloc([128, 8, 512], BF16) for _ in range(2)]
        w2b = [A.alloc([128, 4, D], BF16) for _ in range(2)]
        xb = [A.alloc([128, D], BF16) for _ in range(2)]
        fTb = [A.alloc([128, 8, 128], BF16) for _ in range(2)]
        s1b = [A.alloc([128, 512], BF16) for _ in range(2)]
        ga = [A.alloc([128, 4, 128], BF16) for _ in range(2)]
        ybuf = [A.alloc([128, D], F32) for _ in range(2)]
        print('SBUF B2 top', A.cur, A.top)
        w1rows = w1_d.rearrange("l e r n -> (l e r) n")
        w3rows = w3_d.rearrange("l e r n -> (l e r) n")
        w2rows = w2_d.rearrange("l e r n -> (l e r) n")
        pending = None

        def make_y(b, b_):
            def f():
                for n in range(2):
                    ns = slice(n * 512, (n + 1) * 512)
                    py = PB[4 + n]
                    pyk = 'pb%d' % (4 + n)
                    for j in range(4):
                        mm(py, ga[b_][:, j, :], w2b[b_][:, j, ns], j == 0, j == 3, ['ga%d' % b_, 'w2_%d_%d' % (b_, j)], [pyk])
                    if n == 0:
                        op('dve', lambda e: e.tensor_copy(out=ybuf[b_][:, ns], in_=py), r=[pyk], w=['ybuf%d' % b_])
                    else:
                        op('act', lambda e: e.activation(out=ybuf[b_][:, ns], in_=py, func=AF.Copy), r=[pyk], w=['ybuf%d' % b_])
                P.dma('sp', yg_d[b * 128:(b + 1) * 128, :], ybuf[b_], reads=['ybuf%d' % b_], writes=['yg%d' % b])
            return f

        for b in range(NB):
            b_ = b % 2
            for k in range(8):
                P.idma(w1b[b_][:, k, :], None, w1rows, bass.IndirectOffsetOnAxis(ap=idx1[:, b, k:k + 1], axis=0), reads=['idx1'], writes=['w1_%d_%d' % (b_, k)])
            for k in range(8):
                P.idma(w3b[b_][:, k, :], None, w3rows, bass.IndirectOffsetOnAxis(ap=idx1[:, b, k:k + 1], axis=0), reads=['idx1'], writes=['w3_%d_%d' % (b_, k)])
            for j in range(4):
                P.idma(w2b[b_][:, j, :], None, w2rows, bass.IndirectOffsetOnAxis(ap=idx2[:, b, j:j + 1], axis=0), reads=['idx2'], writes=['w2_%d_%d' % (b_, j)])
            P.dma('sp', xb[b_], xg_d[b * 128:(b + 1) * 128, :], reads=(xg_keys if b < 2 else []), writes=['xb%d' % b_])
            pst = PBH[6 + b_].rearrange("p (k n) -> p k n", k=8)
            for k in range(8):
                op('pe', lambda e: e.transpose(pst[:, k, :], xb[b_][:, k * 128:(k + 1) * 128], identb), r=['xb%d' % b_, 'identb'], w=['pb%d' % (6 + b_)])
            op('act', lambda e: e.activation(out=fTb[b_], in_=pst, func=AF.Copy), r=['pb%d' % (6 + b_)], w=['fTb%d' % b_])
            ph1 = PB[0 + 2 * b_].rearrange("p (j n) -> p j n", j=4)
            ph3 = PB[1 + 2 * b_].rearrange("p (j n) -> p j n", j=4)
            k1 = 'pb%d' % (0 + 2 * b_)
            k3 = 'pb%d' % (1 + 2 * b_)
            for j in range(4):
                for k in range(8):
                    mm(ph1[:, j, :], w1b[b_][:, k, j * 128:(j + 1) * 128], fTb[b_][:, k, :], k == 0, k == 7, ['w1_%d_%d' % (b_, k), 'fTb%d' % b_], [k1])
                for k in range(8):
                    mm(ph3[:, j, :], w3b[b_][:, k, j * 128:(j + 1) * 128], fTb[b_][:, k, :], k == 0, k == 7, ['w3_%d_%d' % (b_, k), 'fTb%d' % b_], [k3])
            if pending is not None:
                pending()
                pending = None
            op('act', lambda e: e.activation(out=s1b[b_], in_=PB[0 + 2 * b_], func=AF.Silu), r=[k1], w=['s1_%d' % b_])
            op('dve', lambda e: e.tensor_tensor(out=ga[b_].rearrange("p j n -> p (j n)"), in0=PB[1 + 2 * b_], in1=s1b[b_], op=ALU.mult), r=[k3, 's1_%d' % b_], w=['ga%d' % b_])
            pending = make_y(b, b_)
        if pending is not None:
            pending()
        P.barrier()
        A.reset(b1_mark)
        y1 = [A.alloc([128, D], F32) for _ in range(2)]
        y2 = [A.alloc([128, D], F32) for _ in range(2)]
        for ti, t in enumerate(act_tiles):
            r_ = 0 if t < 16 else 1
            b_ = ti % 2
            P.idma(y1[b_], None, yg_d, bass.IndirectOffsetOnAxis(ap=slot_i[:, t, 0:1], axis=0), reads=[], writes=['y1_%d' % b_])
            P.idma(y2[b_], None, yg_d, bass.IndirectOffsetOnAxis(ap=slot_i[:, t, 1:2], axis=0), reads=[], writes=['y2_%d' % b_])
            op('dve', lambda e: e.tensor_scalar(out=y1[b_], in0=y1[b_], scalar1=gate2[:, t, 0:1], scalar2=None, op0=ALU.mult), r=['y1_%d' % b_], w=['y1_%d' % b_])
            op('dve', lambda e: e.scalar_tensor_tensor(out=y2[b_], in0=y2[b_], scalar=gate2[:, t, 1:2], in1=y1[b_], op0=ALU.mult, op1=ALU.add), r=['y1_%d' % b_, 'y2_%d' % b_], w=['y2_%d' % b_])
            op('pool', lambda e: e.tensor_tensor(out=y2[b_], in0=y2[b_], in1=gb[:, 1, r_, :], op=ALU.mult), r=['y2_%d' % b_, 'gb'], w=['y2_%d' % b_])
            op('pool', lambda e: e.tensor_tensor(out=xs[:, t, :], in0=xs[:, t, :], in1=y2[b_], op=ALU.add), r=['y2_%d' % b_, 'xs%d' % t], w=['xs%d' % t])
        P.barrier()
        P.mute = False
        A.reset(lay_mark)

    xor = xo_d.rearrange("(t p) d -> p t d", p=128)
    for i in range(4):
        P.dma('sp', xor[:, 4 * i:4 * i + 4, :], xs[:, 4 * i:4 * i + 4, :], reads=['xs%d' % t for t in range(4 * i, 4 * i + 4)], writes=['xo'])
    P.dma('sp', co_d.rearrange("(t p) d -> p t d", p=128), xs[:, 16:18, :], reads=['xs16', 'xs17'], writes=['co'])
    P.finish('sp')
    return nc


def _consts():
    t = np.arange(T)
    rows = (t // 64).astype(np.float32)
    cols = (t % 64).astype(np.float32)
    out = {}
    for d in (64, 48):
        nf = d // 4
        freqs = (10000.0 ** (-np.arange(nf, dtype=np.float32) / nf)).astype(np.float32)
        ar = rows[:, None] * freqs[None, :]
        ac = cols[:, None] * freqs[None, :]
        cos = np.concatenate([np.cos(ar), np.cos(ar), np.cos(ac), np.cos(ac)], axis=1).astype(np.float32)
        sin = np.concatenate([-np.sin(ar), np.sin(ar), -np.sin(ac), np.sin(ac)], axis=1).astype(np.float32)
        cos = np.concatenate([cos, np.ones((C, d), np.float32)], axis=0)
        sin = np.concatenate([sin, np.zeros((C, d), np.float32)], axis=0)
        out['cos%d' % d] = np.ascontiguousarray(cos.reshape(NT, 128, d).transpose(1, 0, 2))
        out['sin%d' % d] = np.ascontiguousarray(sin.reshape(NT, 128, d).transpose(1, 0, 2))
    out['identf'] = np.eye(128, dtype=np.float32)
    s = np.arange(128)
    out['trif'] = (s[:, None] <= s[None, :]).astype(np.float32)
    out['trib'] = (s[:, None] >= s[None, :]).astype(np.float32)
    out['tris'] = (s[:, None] < s[None, :]).astype(np.float32)
    out['thr'] = np.ascontiguousarray(np.broadcast_to((128.0 * np.arange(104, dtype=np.float32))[None, :], (128, 104)))
    out['base1'] = (np.arange(8)[None, :] * 128 + np.arange(128)[:, None]).astype(np.float32)
    out['base2'] = (np.arange(4)[None, :] * 128 + np.arange(128)[:, None]).astype(np.float32)
    return out


_WNAMES = ['norm1_g', 'norm2_g', 'w_mod', 'b_mod', 'w_in', 'w_out', 'diff_q_norm', 'diff_k_norm', 'diff_lambda',
           'diff_subln', 'gqa_q_norm', 'gqa_k_norm', 'mlstm_conv_w', 'mlstm_conv_b', 'mlstm_gate_b', 'mlstm_head_norm',
           'moe_wg', 'moe_bg', 'moe_we', 'moe_be', 'moe_w1', 'moe_w3', 'moe_w2']
_PROG_CACHE = {}


def kernel(x, c, ctx, c_ctx, **w):
    x = np.asarray(x, np.float32)
    ctx = np.asarray(ctx, np.float32)
    c = np.asarray(c, np.float32)
    c_ctx = np.asarray(c_ctx, np.float32)
    w = {k: np.asarray(v, np.float32) for k, v in w.items()}
    consts = _consts()
    NL = N_LAYERS_PER_LAUNCH
    lam_all = np.array([[0.8 - 0.6 * math.exp(-0.3 * l), 1.0 - (0.8 - 0.6 * math.exp(-0.3 * l))] for l in range(DEPTH)], np.float32)
    xcur = [np.ascontiguousarray(x[b]) for b in range(8)]
    ccur = [np.ascontiguousarray(ctx[b]) for b in range(8)]
    ccs = [np.ascontiguousarray(np.stack([c[b], c_ctx], axis=0)) for b in range(8)]
    for l0 in range(0, DEPTH, NL):
        key = (NL, NL == 1)
        if key not in _PROG_CACHE:
            _PROG_CACHE[key] = build_program(NL, do_ctx_last=(NL == 1))
        nc = _PROG_CACHE[key]
        wl = {k: np.ascontiguousarray(w[k][l0:l0 + NL]) for k in _WNAMES}
        wl['lamc'] = np.ascontiguousarray(lam_all[l0:l0 + NL])
        in_maps = []
        for b in range(8):
            m = {'x': xcur[b], 'ctx': ccur[b], 'cc': ccs[b]}
            m.update(wl)
            m.update(consts)
            in_maps.append(m)
        res = run_bass_kernel_spmd(nc, in_maps, core_ids=list(range(8)))
        xcur = [np.asarray(res.results[b]['xo'], np.float32) for b in range(8)]
        ccur = [np.asarray(res.results[b]['co'], np.float32) for b in range(8)]
    return np.stack(xcur, axis=0).astype(np.float32)
```
